# Optimizing a Trainium2 kernel written in Bass

```python
import jax, jax.numpy as jnp
from jax import lax
import numpy as np

D_MODEL = 1024
BATCH = 16
SEQ = 2048
DEPTH = 1

MIX_WIDTH = D_MODEL
MOBA_HEAD_DIM = 64
MOBA_WIDTH = MIX_WIDTH // 2
MOBA_HEADS = MOBA_WIDTH // MOBA_HEAD_DIM
MOBA_BLOCK = 256
MOBA_TOPK = 3
MOBA_Q_CHUNK = 64
GDN_HEAD_DIM = 128
GDN_WIDTH = MIX_WIDTH - MOBA_WIDTH
GDN_HEADS = GDN_WIDTH // GDN_HEAD_DIM
GDN_CONV = 4
GDN_CHUNK = 64
N_GROUPS = 4
EXPERTS_PER_GROUP = 8
EXPERT_TOPK = 2
EXPERT_FF = D_MODEL // 4
RMS_EPS = 1e-6
IN_SPLITS = (MOBA_WIDTH, MOBA_WIDTH, MOBA_WIDTH, 3 * GDN_WIDTH, GDN_WIDTH, GDN_HEADS, GDN_HEADS)
IN_PROJ_WIDTH = 3 * MOBA_WIDTH + 4 * GDN_WIDTH + 2 * GDN_HEADS

kernel_name = 'hybrid_moba_gdn_hmoe'


def rmsnorm(x, gain):
    x32 = x.astype(jnp.float32)
    y = x32 * lax.rsqrt(jnp.mean(x32 * x32, axis=-1, keepdims=True) + RMS_EPS)
    return (y * gain.astype(jnp.float32)).astype(x.dtype)


def l2norm(x):
    return x * lax.rsqrt(jnp.sum(x * x, axis=-1, keepdims=True) + 1e-6)


def moba_attention(q, k, v):
    B, S, H, Dh = q.shape
    nb = -(-S // MOBA_BLOCK)
    pad = nb * MOBA_BLOCK - S
    topk = min(MOBA_TOPK, nb)
    scale = Dh ** -0.5
    q = q.transpose(0, 2, 1, 3)
    kp = jnp.pad(k.transpose(0, 2, 1, 3), ((0, 0), (0, 0), (0, pad), (0, 0)))
    vp = jnp.pad(v.transpose(0, 2, 1, 3), ((0, 0), (0, 0), (0, pad), (0, 0)))
    kb = kp.reshape(B, H, nb, MOBA_BLOCK, Dh)
    vb = vp.reshape(B, H, nb, MOBA_BLOCK, Dh)
    k_mean = jnp.mean(kb.astype(jnp.float32), axis=3)
    b_idx = jnp.arange(B)[:, None, None, None]
    h_idx = jnp.arange(H)[None, :, None, None]
    blk_ids = jnp.arange(nb)
    n_chunks = S // MOBA_Q_CHUNK

    def chunk(c):
        start = c * MOBA_Q_CHUNK
        qc = lax.dynamic_slice_in_dim(q, start, MOBA_Q_CHUNK, axis=2)
        qpos = start + jnp.arange(MOBA_Q_CHUNK)
        own = start // MOBA_BLOCK
        gate = jnp.einsum('bhqd,bhnd->bhqn', qc.astype(jnp.float32), k_mean)
        gate = jnp.where(blk_ids < own, gate, -jnp.inf)
        _, gidx = lax.top_k(gate, topk)
        sel_ok = jnp.arange(topk) < own
        k_sel = kb[b_idx, h_idx, gidx]
        v_sel = vb[b_idx, h_idx, gidx]
        s_sel = jnp.einsum('bhqd,bhqkld->bhqkl', qc, k_sel).astype(jnp.float32) * scale
        s_sel = jnp.where(sel_ok[:, None], s_sel, -jnp.inf)
        k_own = lax.dynamic_slice_in_dim(kb, own, 1, axis=2)[:, :, 0]
        v_own = lax.dynamic_slice_in_dim(vb, own, 1, axis=2)[:, :, 0]
        s_own = jnp.einsum('bhqd,bhld->bhql', qc, k_own).astype(jnp.float32) * scale
        kpos = own * MOBA_BLOCK + jnp.arange(MOBA_BLOCK)
        s_own = jnp.where(kpos[None, :] <= qpos[:, None], s_own, -jnp.inf)
        s = jnp.concatenate([s_sel.reshape(B, H, MOBA_Q_CHUNK, topk * MOBA_BLOCK), s_own], axis=-1)
        p = jax.nn.softmax(s, axis=-1).astype(v.dtype)
        p_sel = p[..., :topk * MOBA_BLOCK].reshape(B, H, MOBA_Q_CHUNK, topk, MOBA_BLOCK)
        p_own = p[..., topk * MOBA_BLOCK:]
        return (jnp.einsum('bhqkl,bhqkld->bhqd', p_sel, v_sel)
                + jnp.einsum('bhql,bhld->bhqd', p_own, v_own))

    out = lax.map(chunk, jnp.arange(n_chunks))
    return out.transpose(1, 0, 3, 2, 4).reshape(B, S, H, Dh)


def causal_depthwise_conv(x, w):
    C = x.shape[-1]
    return lax.conv_general_dilated(
        x, w[:, None, :].astype(x.dtype), window_strides=(1,),
        padding=((GDN_CONV - 1, 0),), dimension_numbers=('NWC', 'WIO', 'NWC'),
        feature_group_count=C)


def gated_delta_rule(q, k, v, g, beta):
    B, S, H, Dk = q.shape
    Dv = v.shape[-1]
    C = GDN_CHUNK
    N = S // C

    def to_chunks(t):
        return t.reshape(B, N, C, H, *t.shape[3:]).swapaxes(2, 3)

    q, k, v, g, beta = (to_chunks(t) for t in (q, k, v, g, beta))
    G = jnp.cumsum(g, axis=-1)
    incl = jnp.tril(jnp.ones((C, C), dtype=bool))
    strict = jnp.tril(jnp.ones((C, C), dtype=bool), k=-1)
    decay = jnp.exp(jnp.where(incl, G[..., :, None] - G[..., None, :], -jnp.inf))
    kk = jnp.einsum('bnhid,bnhjd->bnhij', k, k)
    A = jnp.where(strict, beta[..., :, None] * kk * decay, 0.0)
    eye = jnp.eye(C, dtype=A.dtype)
    gam = jnp.exp(G)
    rhs = jnp.concatenate([beta[..., None] * v, (beta * gam)[..., None] * k], axis=-1)
    sol = lax.linalg.triangular_solve(eye + A, rhs, left_side=True, lower=True, unit_diagonal=True)
    u_v, w = sol[..., :Dv], sol[..., Dv:]
    qk = jnp.einsum('bnhid,bnhjd->bnhij', q, k) * decay
    q_dec = q * gam[..., None]
    k_dec = k * jnp.exp(G[..., -1:] - G)[..., None]
    g_last = jnp.exp(G[..., -1])

    def step(state, xs):
        u_v_c, w_c, qk_c, q_dec_c, k_dec_c, g_last_c = xs
        u = u_v_c - jnp.einsum('bhck,bhkv->bhcv', w_c, state)
        o = jnp.einsum('bhck,bhkv->bhcv', q_dec_c, state) + jnp.einsum('bhij,bhjv->bhiv', qk_c, u)
        new_state = g_last_c[..., None, None] * state + jnp.einsum('bhck,bhcv->bhkv', k_dec_c, u)
        return new_state, o

    xs = tuple(t.swapaxes(0, 1) for t in (u_v, w, qk, q_dec, k_dec, g_last))
    state0 = jnp.zeros((B, H, Dk, Dv), dtype=q.dtype)
    _, o = lax.scan(step, state0, xs)
    return o.transpose(1, 0, 3, 2, 4).reshape(B, S, H, Dv)


def hierarchical_moe(h, w_group, w_router, w_gate, w_up, w_down):
    T = h.shape[0]
    group_logits = jnp.einsum('td,dg->tg', h, w_group).astype(jnp.float32)
    group_prob = jax.nn.softmax(group_logits, axis=-1)
    g_sel = jnp.argmax(group_logits, axis=-1)
    p_group = jnp.take_along_axis(group_prob, g_sel[:, None], axis=-1)
    expert_logits = jnp.einsum('td,de->te', h, w_router).astype(jnp.float32)
    expert_logits = expert_logits.reshape(T, N_GROUPS, EXPERTS_PER_GROUP)
    in_group = jnp.take_along_axis(expert_logits, g_sel[:, None, None], axis=1)[:, 0]
    in_prob = jax.nn.softmax(in_group, axis=-1)
    top_p, top_i = lax.top_k(in_prob, EXPERT_TOPK)
    top_p = top_p / jnp.sum(top_p, axis=-1, keepdims=True)
    within = jnp.sum(jax.nn.one_hot(top_i, EXPERTS_PER_GROUP, dtype=jnp.float32) * top_p[..., None], axis=1)
    combine = (jax.nn.one_hot(g_sel, N_GROUPS, dtype=jnp.float32)[:, :, None]
               * p_group[:, :, None] * within[:, None, :]).astype(h.dtype)
    y = jnp.zeros_like(h)
    for grp in range(N_GROUPS):
        hid = (jax.nn.silu(jnp.einsum('td,edf->tef', h, w_gate[grp]))
               * jnp.einsum('td,edf->tef', h, w_up[grp])) * combine[:, grp, :, None]
        y = y + jnp.einsum('tef,efd->td', hid, w_down[grp])
    return y


def setup_inputs(seed: int = 0) -> dict:
    key = jax.random.key(seed)
    ks = jax.random.split(key, 16)
    f32 = jnp.float32
    D = D_MODEL
    x = jax.random.normal(ks[0], (BATCH, SEQ, D), f32)
    attn_norm = 1.0 + 0.02 * jax.random.normal(ks[1], (DEPTH, D), f32)
    w_in = jax.random.normal(ks[2], (DEPTH, D, IN_PROJ_WIDTH), f32) * D ** -0.5
    conv_w = jax.random.normal(ks[3], (DEPTH, GDN_CONV, 3 * GDN_WIDTH), f32) * GDN_CONV ** -0.5
    A_log = jnp.log(jax.random.uniform(ks[4], (DEPTH, GDN_HEADS), f32, 1.0, 16.0))
    dt = jnp.exp(jax.random.uniform(ks[5], (DEPTH, GDN_HEADS), f32, np.log(1e-3), np.log(1e-1)))
    dt_bias = dt + jnp.log(-jnp.expm1(-dt))
    gdn_norm = 1.0 + 0.02 * jax.random.normal(ks[6], (DEPTH, GDN_HEAD_DIM), f32)
    w_out = jax.random.normal(ks[7], (DEPTH, MIX_WIDTH, D), f32) * MIX_WIDTH ** -0.5
    ffn_norm = 1.0 + 0.02 * jax.random.normal(ks[8], (DEPTH, D), f32)
    w_group = jax.random.normal(ks[9], (DEPTH, D, N_GROUPS), f32) * D ** -0.5
    w_router = jax.random.normal(ks[10], (DEPTH, D, N_GROUPS * EXPERTS_PER_GROUP), f32) * D ** -0.5
    w_gate = jax.random.normal(ks[11], (DEPTH, N_GROUPS, EXPERTS_PER_GROUP, D, EXPERT_FF), f32) * D ** -0.5
    w_up = jax.random.normal(ks[12], (DEPTH, N_GROUPS, EXPERTS_PER_GROUP, D, EXPERT_FF), f32) * D ** -0.5
    w_down = jax.random.normal(ks[13], (DEPTH, N_GROUPS, EXPERTS_PER_GROUP, EXPERT_FF, D), f32) * EXPERT_FF ** -0.5
    final_norm = 1.0 + 0.02 * jax.random.normal(ks[14], (D,), f32)
    return {'x': x, 'attn_norm': attn_norm, 'w_in': w_in, 'conv_w': conv_w, 'A_log': A_log,
            'dt_bias': dt_bias, 'gdn_norm': gdn_norm, 'w_out': w_out, 'ffn_norm': ffn_norm,
            'w_group': w_group, 'w_router': w_router, 'w_gate': w_gate, 'w_up': w_up,
            'w_down': w_down, 'final_norm': final_norm}


def reference(x, attn_norm, w_in, conv_w, A_log, dt_bias, gdn_norm, w_out, ffn_norm,
              w_group, w_router, w_gate, w_up, w_down, final_norm):
    B, S, D = x.shape
    offsets = np.cumsum(IN_SPLITS)[:-1].tolist()
    f32 = jnp.float32
    for l in range(DEPTH):
        h = rmsnorm(x, attn_norm[l])
        proj = jnp.einsum('bsd,de->bse', h, w_in[l])
        mq, mk, mv, gqkv, gz, ga, gb = jnp.split(proj, offsets, axis=-1)
        to_a = lambda t: t.reshape(B, S, MOBA_HEADS, MOBA_HEAD_DIM)
        y_a = moba_attention(to_a(mq), to_a(mk), to_a(mv)).reshape(B, S, MOBA_WIDTH)
        gqkv = jax.nn.silu(causal_depthwise_conv(gqkv, conv_w[l]))
        gq, gk, gv = jnp.split(gqkv, 3, axis=-1)
        to_b = lambda t: t.reshape(B, S, GDN_HEADS, GDN_HEAD_DIM).astype(f32)
        gq = l2norm(to_b(gq)) * GDN_HEAD_DIM ** -0.5
        gk = l2norm(to_b(gk))
        log_decay = -jnp.exp(A_log[l].astype(f32)) * jax.nn.softplus(ga.astype(f32) + dt_bias[l].astype(f32))
        beta = jax.nn.sigmoid(gb.astype(f32))
        o_b = gated_delta_rule(gq, gk, to_b(gv), log_decay, beta)
        o_b = rmsnorm(o_b, gdn_norm[l]) * jax.nn.silu(to_b(gz))
        y_b = o_b.reshape(B, S, GDN_WIDTH).astype(x.dtype)
        mix = jnp.concatenate([y_a, y_b], axis=-1)
        x = x + jnp.einsum('bse,ed->bsd', mix, w_out[l])
        h = rmsnorm(x, ffn_norm[l]).reshape(B * S, D)
        y = hierarchical_moe(h, w_group[l], w_router[l], w_gate[l], w_up[l], w_down[l])
        x = x + y.reshape(B, S, D)
    return rmsnorm(x, final_norm)
```

```python
import numpy as np
import concourse.bass as bass
import concourse.mybir as mybir
from concourse.bass_utils import run_bass_kernel_spmd

F32 = mybir.dt.float32
BF16 = mybir.dt.bfloat16
AF = mybir.ActivationFunctionType
ALU = mybir.AluOpType
AX = mybir.AxisListType

D = 1024
S = 2048
NT = S // 128
NSEQ = 2
EPS = 1e-6
BIG = 30000.0
_ESZ = {F32: 4, BF16: 2}


def _esz(dt):
    return _ESZ.get(dt, 4)


class Trk:
    def __init__(self, nc, sems, dsems):
        self.nc = nc
        self.sem = sems
        self.dsem = dsems
        self.cnt = {e: 0 for e in ("pe", "act", "dve", "pool")}
        self.duse = [0] * len(dsems)
        self.dnext = 0
        self.ops = {e: [] for e in ("pe", "act", "dve", "pool", "sp")}
        self.waited = {e: {} for e in self.ops}
        self.acc = {}
        self.dma_tokens = []

    @staticmethod
    def box(ap):
        t = ap.tensor
        dims = ap.ap
        es = _esz(ap.dtype)
        if "DRAM" in str(ap.space):
            ext = 1
            for st, c in dims:
                ext += (c - 1) * abs(st)
            return (t.name, 0, 1, ap.offset * es, (ap.offset + ext) * es)
        p0 = ap.start_partition()
        p1 = p0 + ap.partition_size()
        row = dims[0][0]
        f0 = ap.offset - p0 * row if row > 0 else ap.offset
        ext = 1
        for st, c in dims[1:]:
            ext += (c - 1) * abs(st)
        if "PSUM" in str(ap.space):
            return (t.name, (p0 // 32) * 32, ((p1 + 31) // 32) * 32, 0, 2048)
        return (t.name, p0, p1, f0 * es, (f0 + ext) * es)

    @staticmethod
    def _ov(a, b):
        return a[1] < b[2] and b[1] < a[2] and a[3] < b[4] and b[3] < a[4]

    @staticmethod
    def _contains(a, b):
        return a[1] <= b[1] and b[2] <= a[2] and a[3] <= b[3] and b[4] <= a[4]

    def _track(self, engine, outs, ins, token):
        deps = set()
        outs = list(outs) + [a for a in ins if "PSUM" in str(a.space)]
        ins = [a for a in ins if "PSUM" not in str(a.space)]
        rb = [self.box(a) for a in ins]
        wb = [self.box(a) for a in outs]
        for b in rb:
            for r in self.acc.get(b[0], ()):
                if r[2] and self._ov(r[0], b):
                    deps.add(r[1])
        for b in wb:
            for r in self.acc.get(b[0], ()):
                if self._ov(r[0], b):
                    deps.add(r[1])
        for b in wb:
            lst = self.acc.setdefault(b[0], [])
            lst[:] = [r for r in lst if not self._contains(b, r[0])]
            lst.append((b, token, True, engine))
        for b in rb:
            lst = self.acc.setdefault(b[0], [])
            rep = False
            if engine != "dma":
                for i, r in enumerate(lst):
                    if (not r[2]) and r[3] == engine and r[0] == b:
                        lst[i] = (b, token, False, engine)
                        rep = True
                        break
            if not rep:
                lst.append((b, token, False, engine))
        deps.discard(token)
        return deps

    def _waits(self, engine, deps):
        out = []
        w = self.waited[engine]
        best = {}
        for key, val in deps:
            if best.get(key, 0) < val:
                best[key] = val
        for key, val in sorted(best.items(), key=lambda d: str(d[0])):
            if key == "pe" and engine == "pe":
                continue
            if w.get(key, 0) >= val:
                continue
            w[key] = val
            sem = self.sem[key] if isinstance(key, str) else self.dsem[key[1]]
            out.append((sem, val))
        return out

    def op(self, engine, fn, outs, ins):
        self.cnt[engine] += 1
        token = (engine, self.cnt[engine])
        deps = self._track(engine, outs, ins, token)
        self.ops[engine].append((self._waits(engine, deps), fn, (self.sem[engine], 1)))

    def dma(self, out, in_, queue="sp", **kw):
        k = self.dnext
        self.dnext = (self.dnext + 1) % len(self.dsem)
        deps = set()
        if self.duse[k] > 0:
            deps.add((("d", k), 16 * self.duse[k]))
        self.duse[k] += 1
        token = (("d", k), 16 * self.duse[k])
        deps |= self._track("dma", [out], [in_], token)
        self.ops[queue].append((self._waits(queue, deps),
                                lambda e, o=out, i=in_, kw=kw: e.dma_start(out=o, in_=i, **kw),
                                (self.dsem[k], 16)))

    def finish(self):
        deps = set()
        for k, u in enumerate(self.duse):
            if u > 0:
                deps.add((("d", k), 16 * u))
        for e in ("pe", "act", "dve", "pool"):
            if self.cnt[e] > 0:
                deps.add((e, self.cnt[e]))
        w = self._waits("sp", deps)
        self.ops["sp"].append((w, None, None))

    def emit(self):
        with self.nc.Block() as block:
            for name, deco in (("sp", block.sync), ("act", block.scalar), ("dve", block.vector),
                               ("pool", block.gpsimd), ("pe", block.tensor)):
                ops = self.ops[name]

                def body(e, ops=ops):
                    for waits, fn, inc in ops:
                        for sem, val in waits:
                            e.wait_ge(sem, val)
                        if fn is None:
                            continue
                        ins = fn(e)
                        if inc is not None:
                            ins.then_inc(inc[0], inc[1])
                deco(body)

    def mm(self, out, lhsT, rhs, start=True, stop=True):
        self.op("pe", lambda e: e.matmul(out, lhsT, rhs, start=start, stop=stop), [out], [lhsT, rhs])

    def tr(self, out, in_, ident):
        self.op("pe", lambda e: e.transpose(out, in_, ident), [out], [in_, ident])

    def act(self, out, in_, func, bias=None, scale=None, accum_out=None):
        kw = {}
        ins = [in_]
        outs = [out]
        if bias is not None:
            kw["bias"] = bias
            if not isinstance(bias, (int, float)):
                ins.append(bias)
        if scale is not None:
            kw["scale"] = scale
            if not isinstance(scale, (int, float)):
                ins.append(scale)
        if accum_out is not None:
            kw["accum_out"] = accum_out
            outs.append(accum_out)
        self.op("act", lambda e: e.activation(out=out, in_=in_, func=func, **kw), outs, ins)

    def ts(self, out, in0, s1, op0, s2=None, op1=None, eng="dve"):
        ins = [in0] + [s for s in (s1, s2) if s is not None and not isinstance(s, (int, float))]
        kw = {}
        if op1 is not None:
            kw["op1"] = op1
        self.op(eng, lambda e: e.tensor_scalar(out=out, in0=in0, scalar1=s1, scalar2=s2, op0=op0, **kw), [out], ins)

    def tt(self, out, in0, in1, op, eng="dve"):
        self.op(eng, lambda e: e.tensor_tensor(out=out, in0=in0, in1=in1, op=op), [out], [in0, in1])

    def stt(self, out, in0, scalar, in1, op0, op1):
        ins = [in0, in1] + ([] if isinstance(scalar, (int, float)) else [scalar])
        self.op("dve", lambda e: e.scalar_tensor_tensor(out=out, in0=in0, scalar=scalar, in1=in1, op0=op0, op1=op1),
                [out], ins)

    def copy(self, out, in_, eng="dve"):
        if eng == "act":
            self.act(out, in_, AF.Copy)
        else:
            self.op(eng, lambda e: e.tensor_copy(out=out, in_=in_), [out], [in_])

    def recip(self, out, in_):
        self.op("dve", lambda e: e.reciprocal(out=out, in_=in_), [out], [in_])

    def red(self, out, in_, op, axis=None):
        ax = AX.X if axis is None else axis
        self.op("dve", lambda e: e.tensor_reduce(out=out, in_=in_, axis=ax, op=op), [out], [in_])

    def max8(self, out, in_):
        self.op("dve", lambda e: e.max(out=out, in_=in_), [out], [in_])

    def memset(self, ap, val, eng="dve"):
        self.op(eng, lambda e: e.memset(ap, val), [ap], [])


CM_NAMES = ["IDN", "ONES", "LTU", "MUS", "MLS", "CA", "CB", "SC", "TRI"]


def _const_mats():
    i = np.arange(128)
    same = (i[:, None] // 64) == (i[None, :] // 64)
    m = {}
    m["IDN"] = np.eye(128)
    m["ONES"] = np.ones((128, 128))
    m["LTU"] = (same & (i[:, None] <= i[None, :]))
    m["MUS"] = (same & (i[:, None] < i[None, :]))
    m["MLS"] = (same & (i[:, None] > i[None, :]))
    m["CA"] = np.broadcast_to((i[:, None] < 64), (128, 128))
    m["CB"] = np.broadcast_to((i[:, None] >= 64), (128, 128))
    m["SC"] = same
    m["TRI"] = (i[:, None] <= i[None, :])
    return np.concatenate([np.asarray(m[n], dtype=np.float32) for n in CM_NAMES], axis=1)


def _attn_consts():
    neg = np.zeros((4, 8, 8), np.float32)
    cst = np.zeros((8, 8, 8), np.float32)
    base = np.zeros((8, 8, 8), np.float32)
    j = np.arange(8)
    for own in range(8):
        cst[own] = np.where(j == own, 0.0, -BIG)[None, :]
        base[own] = np.where(j <= own, 0.0, -BIG)[None, :]
        if own >= 4:
            neg[own - 4] = np.where(j < own, 0.0, -1e30)[None, :]
    row = np.concatenate([neg.reshape(-1), cst.reshape(-1), base.reshape(-1)])
    return np.broadcast_to(row[None, :], (128, row.size)).astype(np.float32).copy()


class _Done(Exception):
    pass


def build_program(stage=0, nseq=NSEQ):
    nc = bass.Bass("TRN2", target_bir_lowering=False)
    dbg_d = nc.dram_tensor("dbg", [16, 128, 2048], F32, kind="ExternalOutput").ap() if stage else None

    def din(name, shape):
        return nc.dram_tensor(name, list(shape), F32, kind="ExternalInput").ap()

    x_d = din("x", [NSEQ, S, D])
    win_d = din("w_in", [128, 8, 3592])
    wout_d = din("w_out", [128, 8, 1024])
    wr_d = din("w_r", [128, 8, 36])
    ne_decl = 32 if stage in (0, 6, 7, 8) else 1
    wg_d = din("w_gate", [ne_decl, 128, 8 * 256])
    wu_d = din("w_up", [ne_decl, 128, 8 * 256])
    wd_d = din("w_down", [ne_decl, 128, 2 * 1024])
    cm_d = din("cm", [128, 9 * 128])
    ac_d = din("ac", [128, 256 + 512 + 512])
    kind_d = din("kind", [8, S])
    gains_d = din("gains", [128, 16])
    fg_d = din("fgain", [128, D])
    gn_d = din("gn4", [128, 512])
    cw_d = din("cw", [128, 48])
    dtb_d = din("dtb", [128, 64])
    alog_d = din("alog", [128, 64])
    sele_d = din("sele", [32, 32 * 128])
    out_d = nc.dram_tensor("out", [NSEQ, S, D], F32, kind="ExternalOutput").ap()

    import contextlib
    es = contextlib.ExitStack()
    with es:
        def sb(name, shape, dt=F32):
            return es.enter_context(nc.sbuf_tensor("sb_" + name, list(shape), dt))

        def sem(name):
            return es.enter_context(nc.semaphore(name))

        sems = {e: sem("s_" + e) for e in ("pe", "act", "dve", "pool")}
        dsems = [sem("d%d" % i) for i in range(24)]
        T = Trk(nc, sems, dsems)

        cm = sb("cm", [128, 9 * 128])
        CM = {n: cm[:, i * 128:(i + 1) * 128] for i, n in enumerate(CM_NAMES)}
        ac = sb("ac", [128, 1280])
        negmask = ac[:, 0:256].rearrange("p (o c) -> p o c", o=4)
        cstb = ac[:, 256:768].rearrange("p (o c) -> p o c", o=8)
        basebias = ac[:, 768:1280].rearrange("p (o c) -> p o c", o=8)
        idb = sb("idb", [128, 128], BF16)
        trib = sb("trib", [128, 128], BF16)
        idn4 = sb("idn4", [128, 4, 128])
        ltu4 = sb("ltu4", [128, 4, 128])
        gains = sb("gains", [128, 16])
        gn4 = sb("gn4", [128, 512])
        cw = sb("cw", [128, 48])
        dtb = sb("dtb", [128, 64])
        nega = sb("nega", [128, 64])
        gsm = sb("gsm", [128, 96])
        small = sb("small", [128, 256])
        xt = [sb("xt%d" % i, [128, D]) for i in range(2)]
        xn = sb("xn", [128, D], BF16)
        wst = [sb("wst%d" % i, [128, 8, 128]) for i in range(2)]
        wbf = [sb("wbf%d" % i, [128, 8, 128], BF16) for i in range(2)]
        hT = sb("hT", [128, 8 * S], BF16)
        mixT_t = sb("mixT", [128, 8 * S], BF16)
        X = sb("X", [128, 22528])
        P = [es.enter_context(nc.psum_tensor("ps%d" % i, [128, 512], F32)) for i in range(8)]

        hT3 = hT[:, :].rearrange("p (c t) -> p c t", c=8)
        mixT = mixT_t[:, :].rearrange("p (c t) -> p c t", c=8)

        def carve(arena, off_b, nbytes, dt, parts=128):
            aes = _esz(arena.dtype)
            v = arena[0:parts, off_b // aes:(off_b + nbytes) // aes]
            if dt != arena.dtype:
                v = v.bitcast(dt)
            return v

        def pbf(bank):
            return P[bank][:, :].bitcast(BF16)

        sc_i = [0]

        def sc(n):
            if sc_i[0] + n > 256:
                sc_i[0] = 0
            a = small[:, sc_i[0]:sc_i[0] + n]
            sc_i[0] += n
            return a

        T.dma(cm[:, :], cm_d[:, :])
        T.dma(ac[:, :], ac_d[:, :])
        T.dma(gains[:, :], gains_d[:, :])
        T.dma(gn4[:, :], gn_d[:, :])
        T.dma(cw[:, :], cw_d[:, :])
        T.dma(dtb[:, :], dtb_d[:, :])
        T.dma(nega[:, :], alog_d[:, :])
        T.copy(idb[:, :], CM["IDN"])
        T.copy(trib[:, :], CM["TRI"])
        for h in range(4):
            T.copy(idn4[:, h, :], CM["IDN"])
            T.copy(ltu4[:, h, :], CM["LTU"])
        T.act(nega[:, :], nega[:, :], AF.Exp)
        T.ts(nega[:, :], nega[:, :], -1.0, ALU.mult)

        wload_i = [0]

        def load_w(src_ap, ncols):
            i = wload_i[0] % 2
            wload_i[0] += 1
            T.dma(wst[i][:, :, 0:ncols], src_ap)
            T.copy(wbf[i][:, :, 0:ncols], wst[i][:, :, 0:ncols], eng="pool")
            return wbf[i]

        def rms_rstd(src, n, junk):
            ss = sc(1)
            T.act(junk, src, AF.Square, accum_out=ss)
            sd = sc(1)
            T.act(sd, ss, AF.Sqrt, bias=epsc[:, 0:1], scale=1.0 / n)
            rs = sc(1)
            T.recip(rs, sd)
            return rs

        epsc = sb("epsc", [128, 2])
        T.memset(epsc[:, 0:1], EPS)
        T.memset(epsc[:, 1:2], 1.0)

        def norm_T(src, gcol0, dst3, tok0):
            rs = rms_rstd(src, D, xn[:, :])
            T.ts(xn[:, :], src, rs, ALU.mult)
            pt = pbf(7).rearrange("p (c t) -> p c t", c=8)
            for c in range(8):
                T.tr(pt[:, c, :], xn[:, c * 128:(c + 1) * 128], idb[:, :])
            for c in range(8):
                T.act(dst3[:, c, tok0:tok0 + 128], pt[:, c, :], AF.Copy, scale=gains[:, gcol0 + c:gcol0 + c + 1])

        dbgn = [0]
        dbgst = sb("dbgst", [128, 2048]) if stage else None

        def ck(k, aps):
            if stage != k:
                return
            for a in aps:
                n = a.shape[-1]
                p = a.shape[0]
                T.copy(dbgst[0:p, 0:n], a)
                T.dma(dbg_d[dbgn[0], 0:p, 0:n], dbgst[0:p, 0:n])
                dbgn[0] += 1
            raise _Done()

        try:
          for s in range(nseq):
              for t in range(NT):
                  xb = xt[t % 2]
                  T.dma(xb[:, :], x_d[s, t * 128:(t + 1) * 128, :])
                  norm_T(xb[:, :], 0, hT3, t * 128)

              ck(1, [hT3[:, 0, :], hT3[:, 7, :]])
              qa = carve(X, 0, 32768, BF16, parts=72).rearrange("p (h t) -> p h t", h=8)
              ka = carve(X, 32768, 32768, BF16, parts=72).rearrange("p (h t) -> p h t", h=8)
              vaug = carve(X, 65536, 16 * 8 * 65 * 2, BF16).rearrange("p (t h e) -> p t h e", t=16, h=8)
              o2 = 65536 + 16 * 8 * 65 * 2
              ya = [carve(X, o2 + i * 1024, 1024, BF16) for i in range(4)]
              o2 += 4096
              pTb = [carve(X, o2 + i * 1024, 1024, BF16) for i in range(2)]
              o2 += 2048
              kms = carve(X, o2, 256, F32, parts=64).rearrange("p (h j) -> p h j", h=8)
              o2 += 256
              kmb = carve(X, o2, 128, BF16, parts=64).rearrange("p (h j) -> p h j", h=8)
              o2 += 128
              gm = carve(X, o2, 256, F32)
              o2 += 256
              top = carve(X, o2, 256, F32).rearrange("p (h j) -> p h j", h=8)
              o2 += 256
              selb = carve(X, o2, 256, F32)
              o2 += 256
              sbias = carve(X, o2, 128, BF16)
              o2 += 128
              stg = carve(X, o2, 256, BF16, parts=64)
              o2 += 256

              T.memset(vaug[:, :, :, 64:65], 1.0)
              for half in range(2):
                  T.dma(xt[0][64:72, :], kind_d[:, half * 1024:(half + 1) * 1024])
                  for h in range(8):
                      T.copy(ka[64:72, h, half * 1024:(half + 1) * 1024], xt[0][64:72, :], eng="pool")

              for which, dst in ((0, qa), (1, ka)):
                  for g2 in range(4):
                      c0 = which * 512 + g2 * 128
                      w = load_w(win_d[:, :, c0:c0 + 128], 128)
                      for hh in range(2):
                          h = g2 * 2 + hh
                          for tc in range(4):
                              ps = P[(h * 4 + tc) % 2]
                              for c in range(8):
                                  T.mm(ps[0:64, :], w[:, c, hh * 64:(hh + 1) * 64], hT3[:, c, tc * 512:(tc + 1) * 512],
                                       start=(c == 0), stop=(c == 7))
                              T.copy(dst[0:64, h, tc * 512:(tc + 1) * 512], ps[0:64, :], eng=("act" if tc % 2 else "dve"))
              for g2 in range(4):
                  c0 = 1024 + g2 * 128
                  w = load_w(win_d[:, :, c0:c0 + 128], 128)
                  for t in range(NT):
                      ps = P[t % 2]
                      for c in range(8):
                          T.mm(ps[:, 0:128], hT3[:, c, t * 128:(t + 1) * 128], w[:, c, :], start=(c == 0), stop=(c == 7))
                      T.copy(vaug[:, t, g2 * 2:g2 * 2 + 2, 0:64], ps[:, 0:128].rearrange("p (h e) -> p h e", h=2),
                             eng=("act" if t % 2 else "dve"))
              for h in range(8):
                  T.red(kms[:, h, :], ka[0:64, h, :].rearrange("p (j l) -> p j l", j=8), ALU.add)
              T.ts(kmb[:, :, :], kms[:, :, :], 1.0 / 256.0, ALU.mult)
              for qt in range(NT):
                  own = qt // 2
                  if own >= 4:
                      psg = P[6]
                      for h in range(8):
                          T.mm(psg[:, h * 8:(h + 1) * 8], qa[0:64, h, qt * 128:(qt + 1) * 128], kmb[:, h, :])
                      T.tt(gm, psg[:, 0:64], negmask[:, own - 4, :], ALU.add)
                      gm3 = gm.rearrange("p (h j) -> p h j", h=8)
                      for h in range(8):
                          T.max8(top[:, h, :], gm3[:, h, :])
                      for h in range(8):
                          T.ts(selb[:, h * 8:(h + 1) * 8], gm3[:, h, :], top[:, h, 2:3], ALU.is_ge)
                      T.stt(sbias, selb, BIG, cstb[:, own, :], ALU.mult, ALU.add)
                  else:
                      T.copy(sbias, basebias[:, own, :])
                  pt = pbf(7)
                  T.tr(pt[0:64, 0:128], sbias, idb[:, :])
                  T.copy(stg, pt[0:64, 0:128])
                  for h in range(8):
                      T.dma(qa[64:72, h, qt * 128:(qt + 1) * 128], stg[h * 8:(h + 1) * 8, :])
              ck(2, [qa[:, 0, :], qa[:, 5, :], ka[:, 0, :], ka[:, 5, :], vaug[:, 3, :, :].rearrange('p h e -> p (h e)'), kmb[:, :, :].rearrange('p h j -> p (h j)')])
              for qc in range(4):
                  for h in range(8):
                      for kt in range(4 * qc + 4):
                          i0 = max(0, kt - 4 * qc)
                          ncol = (4 - i0) * 128
                          c0 = qc * 512 + i0 * 128
                          ps = P[kt % 2]
                          T.mm(ps[:, 0:ncol], ka[0:72, h, kt * 128:(kt + 1) * 128], qa[0:72, h, c0:c0 + ncol])
                          pT = pTb[kt % 2]
                          T.act(pT[:, 0:ncol], ps[:, 0:ncol], AF.Exp, scale=0.125)
                          if kt >= 4 * qc:
                              T.tt(pT[:, 0:128], pT[:, 0:128], trib[:, :], ALU.mult, eng="pool")
                          for i in range(i0, 4):
                              qt = 4 * qc + i
                              T.mm(P[2 + i][:, 0:65], pT[:, (i - i0) * 128:(i - i0 + 1) * 128], vaug[:, kt, h, :],
                                   start=(kt == 0), stop=(kt == qt))
                      for i in range(4):
                          rec = sc(1)
                          T.recip(rec, P[2 + i][:, 64:65])
                          T.act(ya[i][:, h * 64:(h + 1) * 64], P[2 + i][:, 0:64], AF.Copy, scale=rec)
                  for i in range(4):
                      qt = 4 * qc + i
                      pt = pbf(7).rearrange("p (c t) -> p c t", c=8)
                      for c in range(4):
                          T.tr(pt[:, c, :], ya[i][:, c * 128:(c + 1) * 128], idb[:, :])
                      T.copy(mixT[:, 0:4, qt * 128:(qt + 1) * 128], pt[:, 0:4, :])

              ck(3, [mixT[:, 0, :], mixT[:, 3, :]])
              qnT = carve(X, 0, 16384, BF16).rearrange("p (h t) -> p h t", h=4)
              knT = carve(X, 16384, 16384, BF16).rearrange("p (h t) -> p h t", h=4)
              vT = carve(X, 32768, 16384, BF16).rearrange("p (h t) -> p h t", h=4)
              zs = carve(X, 49152, 16384, BF16).rearrange("p (t e) -> p t e", t=16)
              o2 = 65536
              pre = carve(X, o2, 2052 * 4, F32)
              o2 += 2052 * 4
              cacc = carve(X, o2, 8192, F32)
              o2 += 8192
              tsq = carve(X, o2, 2048, F32)
              o2 += 2048
              tsd = carve(X, o2, 2048, F32)
              o2 += 2048
              gt = carve(X, o2, 256, F32).rearrange("p (t h) -> p t h", t=16)
              o2 += 256
              bet = carve(X, o2, 256, F32).rearrange("p (t h) -> p t h", t=16)
              o2 += 256
              xab = carve(X, o2, 256, F32)
              o2 += 256
              T.memset(pre[:, 0:3], 0.0)

              for ch in range(12):
                  c0 = 1536 + ch * 128
                  w = load_w(win_d[:, :, c0:c0 + 128], 128)
                  for tc in range(4):
                      ps = P[tc % 2]
                      for c in range(8):
                          T.mm(ps[:, :], w[:, c, :], hT3[:, c, tc * 512:(tc + 1) * 512], start=(c == 0), stop=(c == 7))
                      T.copy(pre[:, 3 + tc * 512:3 + (tc + 1) * 512], ps[:, :], eng="act")
                  T.ts(cacc, pre[:, 0:S], cw[:, ch * 4:ch * 4 + 1], ALU.mult)
                  for i in range(1, 4):
                      T.stt(cacc, pre[:, i:i + S], cw[:, ch * 4 + i:ch * 4 + i + 1], cacc, ALU.mult, ALU.add)
                  kind, h = ch // 4, ch % 4
                  if kind == 2:
                      T.act(vT[:, h, :], cacc, AF.Silu)
                  else:
                      T.act(cacc, cacc, AF.Silu)
                      dst = qnT if kind == 0 else knT
                      scl = (128.0 ** -0.5) if kind == 0 else 1.0
                      for tc in range(4):
                          sl = slice(tc * 512, (tc + 1) * 512)
                          T.act(tsq, cacc[:, sl], AF.Square)
                          psn = P[2 + tc % 2]
                          T.mm(psn[:, :], CM["ONES"], tsq)
                          T.act(tsd, psn[:, :], AF.Sqrt, bias=epsc[:, 0:1], scale=1.0)
                          T.recip(tsd, tsd)
                          T.stt(dst[:, h, sl], cacc[:, sl], scl, tsd, ALU.mult, ALU.mult)
              for g2 in range(4):
                  c0 = 3072 + g2 * 128
                  w = load_w(win_d[:, :, c0:c0 + 128], 128)
                  for t in range(NT):
                      ps = P[t % 2]
                      for c in range(8):
                          T.mm(ps[:, 0:128], hT3[:, c, t * 128:(t + 1) * 128], w[:, c, :], start=(c == 0), stop=(c == 7))
                      T.act(zs[:, t, g2 * 128:(g2 + 1) * 128], ps[:, 0:128], AF.Silu)
              w = load_w(win_d[:, :, 3584:3592], 8)
              psab = P[4]
              for t in range(NT):
                  for c in range(8):
                      T.mm(psab[:, t * 8:(t + 1) * 8], hT3[:, c, t * 128:(t + 1) * 128], w[:, c, 0:8],
                           start=(c == 0), stop=(c == 7))
              pab3 = psab[:, 0:128].rearrange("p (t e) -> p t e", t=16)
              xab3 = xab.rearrange("p (t h) -> p t h", t=16)
              T.tt(xab3, pab3[:, :, 0:4], dtb[:, :].rearrange("p (t h) -> p t h", t=16), ALU.add)
              T.act(xab, xab, AF.Exp)
              T.act(xab, xab, AF.Ln, bias=epsc[:, 1:2], scale=1.0)
              T.tt(gt, xab3, nega[:, :].rearrange("p (t h) -> p t h", t=16), ALU.mult)
              T.act(bet, pab3[:, :, 4:8], AF.Sigmoid)

              ck(4, [qnT[:, 0, :], knT[:, 2, :], vT[:, 1, :], zs[:, 5, :], gt[:, :, :].rearrange('p t h -> p (t h)'), bet[:, :, :].rearrange('p t h -> p (t h)')])
              hoff = [0]

              def hc(nbytes, dt, shape4=True):
                  v = carve(hT, hoff[0], nbytes, dt)
                  hoff[0] += nbytes
                  return v.rearrange("p (h e) -> p h e", h=4) if shape4 else v

              gbc = hc(2048, F32)
              dt4 = hc(2048, F32)
              dl4 = hc(2048, F32)
              A4 = hc(2048, F32)
              qkm4 = hc(1024, BF16)
              qb = [hc(1024, BF16) for _ in range(2)]
              qtb = [hc(1024, BF16) for _ in range(2)]
              pb = [hc(1024, BF16) for _ in range(2)]
              bgk = hc(1024, BF16)
              kdec = hc(1024, BF16)
              bv = hc(1024, BF16)
              uv = hc(2048, F32)
              wT = hc(1024, BF16)
              ub = hc(1024, BF16)
              o1s = hc(2048, F32)
              ot = hc(2048, F32)
              S32 = hc(2048, F32)
              Sb = hc(1024, BF16)
              zg = hc(2048, F32)
              yb = hc(1024, BF16)
              Gs = gsm[:, 0:16]
              ex = gsm[:, 16:48]
              assert hoff[0] <= 32768
              T.memset(S32[:, :, :], 0.0)
              T.memset(Sb[:, :, :], 0.0)
              gam, glab, kds, bg, d1 = ex[:, 0:4], ex[:, 4:12], ex[:, 12:16], ex[:, 16:20], ex[:, 20:24]
              ss4, rs4 = ex[:, 24:28], ex[:, 28:32]
              kdsA, kdsB = gsm[:, 48:52], gsm[:, 52:56]
              kdecB = dl4[:, :, :].rearrange('p h e -> p (h e)')[:, 0:256].bitcast(BF16).rearrange('p (h e) -> p h e', h=4)
              T.memset(ub[:, :, :], 0.0)

              def b4(bank):
                  return P[bank][:, :].rearrange("p (h e) -> p h e", h=4)

              def b4bf(bank):
                  return pbf(bank)[:, 0:512].rearrange("p (h e) -> p h e", h=4)

              for t in range(NT):
                  tl = slice(t * 128, (t + 1) * 128)
                  g_t, b_t = gt[:, t, :], bet[:, t, :]
                  psG = P[0]
                  for i, mname in enumerate(("LTU", "SC", "CA", "CB")):
                      T.mm(psG[:, i * 4:(i + 1) * 4], CM[mname], g_t)
                  T.copy(Gs, psG[:, 0:16])
                  ck(41, [Gs])
                  T.act(gam, Gs[:, 0:4], AF.Exp)
                  T.act(glab, Gs[:, 8:16], AF.Exp)
                  T.tt(d1, Gs[:, 4:8], Gs[:, 0:4], ALU.subtract)
                  T.act(kds, d1, AF.Exp)
                  T.tt(bg, b_t, gam, ALU.mult)
                  T.ts(kdsA, kds, CM["CA"][:, 0:1], ALU.mult)
                  T.ts(kdsB, kds, CM["CB"][:, 0:1], ALU.mult)
                  psR = b4(1)
                  for h in range(4):
                      T.ts(gbc[:, h, :], CM["ONES"], g_t[:, h:h + 1], ALU.mult)
                      T.mm(psR[:, h, :], gbc[:, h, :], CM["LTU"])
                  for h in range(4):
                      T.ts(dt4[:, h, :], psR[:, h, :], Gs[:, h:h + 1], ALU.subtract, 0.0, ALU.min)
                      T.ts(dl4[:, h, :], psR[:, h, :], Gs[:, h:h + 1], ALU.subtract, 0.0, ALU.max)
                  T.act(dt4[:, :, :], dt4[:, :, :], AF.Exp)
                  T.act(dl4[:, :, :], dl4[:, :, :], AF.Exp, scale=-1.0)
                  psK, psQ = b4(2), b4(3)
                  for h in range(4):
                      T.mm(psK[:, h, :], knT[:, h, tl], knT[:, h, tl])
                      T.mm(psQ[:, h, :], knT[:, h, tl], qnT[:, h, tl])
                  T.tt(A4[:, :, :], psK, dl4[:, :, :], ALU.mult)
                  for h in range(4):
                      T.stt(A4[:, h, :], A4[:, h, :], b_t[:, h:h + 1], CM["MLS"], ALU.mult, ALU.mult)
                  T.tt(dt4[:, :, :], psQ, dt4[:, :, :], ALU.mult)
                  T.tt(qkm4[:, :, :], dt4[:, :, :], ltu4[:, :, :], ALU.mult)
                  psN = b4(4)
                  for h in range(4):
                      T.tr(psN[:, h, :], A4[:, h, :], CM["IDN"])
                  T.copy(qtb[0][:, :, :], A4[:, :, :], eng="pool")
                  T.copy(qb[0][:, :, :], psN, eng="act")
                  T.tt(pb[0][:, :, :], idn4[:, :, :], psN, ALU.subtract)
                  cq, cp = 0, 0
                  for k in range(1, 6):
                      psA = b4(5)
                      for h in range(4):
                          T.mm(psA[:, h, :], qb[cq][:, h, :], qtb[cq][:, h, :])
                      T.copy(qtb[1 - cq][:, :, :], psA, eng="act")
                      if k < 5:
                          psB = b4(6)
                          for h in range(4):
                              T.mm(psB[:, h, :], qtb[cq][:, h, :], qb[cq][:, h, :])
                          T.copy(qb[1 - cq][:, :, :], psB)
                      psC = b4(0)
                      for h in range(4):
                          T.mm(psC[:, h, :], qtb[1 - cq][:, h, :], pb[cp][:, h, :])
                      T.tt(pb[1 - cp][:, :, :], pb[cp][:, :, :], psC, ALU.add)
                      cq, cp = 1 - cq, 1 - cp
                  TT = pb[cp]
                  ck(42, [TT[:, 0, :], TT[:, 3, :], qkm4[:, 1, :]])
                  ptk = pbf(7)[:, 0:512].rearrange('p (h e) -> p h e', h=4)
                  ptv = pbf(7)[:, 512:1024].rearrange('p (h e) -> p h e', h=4)
                  for h in range(4):
                      T.tr(ptk[:, h, :], knT[:, h, tl], idb[:, :])
                      T.tr(ptv[:, h, :], vT[:, h, tl], idb[:, :])
                  ck(46, [TT[:, 0, :]])
                  for h in range(4):
                      T.act(bgk[:, h, :], ptk[:, h, :], AF.Copy, scale=bg[:, h:h + 1])
                      T.ts(kdec[:, h, :], ptk[:, h, :], kdsA[:, h:h + 1], ALU.mult)
                      T.ts(kdecB[:, h, :], ptk[:, h, :], kdsB[:, h:h + 1], ALU.mult)
                      T.act(bv[:, h, :], ptv[:, h, :], AF.Copy, scale=b_t[:, h:h + 1])
                  ck(45, [bgk[:, 2, :], kdec[:, 0, :], kdecB[:, 3, :], bv[:, 1, :]])
                  psU, psW = b4(3), b4(4)
                  for h in range(4):
                      T.mm(psU[:, h, :], TT[:, h, :], bv[:, h, :])
                      T.mm(psW[:, h, :], bgk[:, h, :], TT[:, h, :])
                  T.copy(uv[:, :, :], psU)
                  T.copy(wT[:, :, :], psW, eng="act")
                  ck(44, [uv[:, 0, :], wT[:, 1, :], bgk[:, 2, :], kdecB[:, 3, :]])
                  for xi in range(2):
                      r = slice(xi * 64, (xi + 1) * 64)
                      psWS, psO1, psO2, psS = b4(5), b4(6), b4(0), b4(1)
                      kd = kdec if xi == 0 else kdecB
                      for h in range(4):
                          T.mm(psWS[:, h, :], wT[:, h, :], Sb[:, h, :])
                      T.tt(ub[r, :, :], uv[r, :, :], psWS[r, :, :], ALU.subtract)
                      for h in range(4):
                          T.mm(psO1[:, h, :], qnT[:, h, tl], Sb[:, h, :])
                          T.mm(psO2[:, h, :], qkm4[:, h, :], ub[:, h, :])
                          T.mm(psS[:, h, :], kd[:, h, :], ub[:, h, :])
                      for h in range(4):
                          T.act(o1s[r, h, :], psO1[r, h, :], AF.Copy, scale=gam[r, h:h + 1])
                      T.tt(ot[r, :, :], o1s[r, :, :], psO2[r, :, :], ALU.add)
                      for h in range(4):
                          T.stt(S32[:, h, :], S32[:, h, :], glab[:, xi * 4 + h:xi * 4 + h + 1], psS[:, h, :],
                                ALU.mult, ALU.add)
                      T.copy(Sb[:, :, :], S32[:, :, :], eng="pool")
                  ck(43, [ot[:, 0, :], ot[:, 3, :], S32[:, 2, :]])
                  for h in range(4):
                      T.act(o1s[:, h, :], ot[:, h, :], AF.Square, accum_out=ss4[:, h:h + 1])
                  T.act(rs4, ss4, AF.Sqrt, bias=epsc[:, 0:1], scale=1.0 / 128.0)
                  T.recip(rs4, rs4)
                  T.tt(zg[:, :, :], zs[:, t, :].rearrange("p (h e) -> p h e", h=4),
                       gn4[:, :].rearrange("p (h e) -> p h e", h=4), ALU.mult)
                  for h in range(4):
                      T.stt(yb[:, h, :], ot[:, h, :], rs4[:, h:h + 1], zg[:, h, :], ALU.mult, ALU.mult)
                  pty = b4bf(7)
                  for h in range(4):
                      T.tr(pty[:, h, :], yb[:, h, :], idb[:, :])
                  T.copy(mixT[:, 4:8, tl], pty)

              ck(5, [mixT[:, 4, :], mixT[:, 7, :]])
              yacc = carve(X, 0, 65536, F32).rearrange("p (t d) -> p t d", t=16)
              wo = carve(X, 65536, 16384, BF16).rearrange("p (c d) -> p c d", c=8)
              for g2 in range(8):
                  w = load_w(wout_d[:, :, g2 * 128:(g2 + 1) * 128], 128)
                  T.copy(wo[:, :, g2 * 128:(g2 + 1) * 128], w[:, :, :], eng="pool")
              for t in range(NT):
                  tl = slice(t * 128, (t + 1) * 128)
                  xb = xt[t % 2]
                  T.dma(xb[:, :], x_d[s, tl, :])
                  for half in range(2):
                      ps = P[half + 2 * (t % 2)]
                      for c in range(8):
                          T.mm(ps[:, :], mixT[:, c, tl], wo[:, c, half * 512:(half + 1) * 512], start=(c == 0), stop=(c == 7))
                      T.tt(yacc[:, t, half * 512:(half + 1) * 512], xb[:, half * 512:(half + 1) * 512], ps[:, :], ALU.add)
                  norm_T(yacc[:, t, :], 8, hT3, t * 128)

              ck(6, [yacc[:, 0, :], yacc[:, 9, :], hT3[:, 2, :]])
              h2T = hT3
              o2 = 65536
              wr = carve(X, o2, 8 * 36 * 2, BF16).rearrange("p (c e) -> p c e", c=8)
              o2 += 8 * 36 * 2
              combT = carve(X, o2, 4096, BF16, parts=32)
              o2 += 4096
              lg = carve(X, o2, 144, F32)
              o2 += 144
              lm = carve(X, o2, 128, F32)
              o2 += 128
              ee = carve(X, o2, 128, F32)
              o2 += 128
              selm = carve(X, o2, 128, F32)
              o2 += 128
              comb = carve(X, o2, 128, F32)
              o2 += 128
              t8 = carve(X, o2, 32, F32)
              o2 += 32
              sg = carve(X, o2, 4096, F32).rearrange("p (f t) -> p f t", f=2)
              o2 += 4096
              hid = [carve(X, o2 + i * 2048, 2048, BF16).rearrange("p (f t) -> p f t", f=2) for i in range(2)]
              o2 += 4096
              wgb = [carve(X, o2 + i * 4096, 4096, BF16).rearrange("p (c f) -> p c f", c=8) for i in range(2)]
              o2 += 8192
              assert o2 <= 22528 * 4
              wub = [carve(mixT_t, i * 4096, 4096, BF16).rearrange("p (c f) -> p c f", c=8) for i in range(2)]
              wdb = [carve(mixT_t, 8192 + i * 4096, 4096, BF16).rearrange("p (c d) -> p c d", c=2) for i in range(2)]
              selst = carve(mixT_t, 16384, 8192, BF16, parts=32).rearrange("p (e m) -> p e m", e=32)
              wstage = carve(mixT_t, 24576, 8192, F32)

              w = load_w(wr_d[:, :, :], 36)
              T.copy(wr[:, :, :], w[:, :, 0:36], eng="pool")
              for half in range(2):
                  T.dma(wstage[0:32, :], sele_d[:, half * 2048:(half + 1) * 2048])
                  T.copy(selst[:, half * 16:(half + 1) * 16, :],
                         wstage[0:32, :].rearrange("p (e m) -> p e m", e=16), eng="pool")
              for t in range(NT):
                  tl = slice(t * 128, (t + 1) * 128)
                  psL = P[t % 2]
                  for c in range(8):
                      T.mm(psL[:, 0:36], h2T[:, c, tl], wr[:, c, :], start=(c == 0), stop=(c == 7))
                  T.copy(lg, psL[:, 0:36])
                  gmax, ngm, sume, pg = sc(1), sc(1), sc(1), sc(1)
                  T.red(gmax, lg[:, 0:4], ALU.max)
                  T.ts(ngm, gmax, -1.0, ALU.mult)
                  eg = sc(4)
                  T.act(eg, lg[:, 0:4], AF.Exp, bias=ngm, scale=1.0, accum_out=sume)
                  T.recip(pg, sume)
                  gsel = sc(4)
                  T.ts(gsel, lg[:, 0:4], gmax, ALU.is_ge, -1.0, ALU.add)
                  T.ts(gsel, gsel, 1e30, ALU.mult)
                  for g in range(4):
                      T.ts(lm[:, g * 8:(g + 1) * 8], lg[:, 4 + g * 8:12 + g * 8], gsel[:, g:g + 1], ALU.add)
                  T.max8(t8, lm)
                  nt1 = sc(1)
                  T.ts(nt1, t8[:, 0:1], -1.0, ALU.mult)
                  T.act(ee, lm, AF.Exp, bias=nt1, scale=1.0)
                  T.ts(selm, lm, t8[:, 1:2], ALU.is_ge)
                  T.tt(ee, ee, selm, ALU.mult)
                  den, scl2 = sc(1), sc(1)
                  T.red(den, ee, ALU.add)
                  T.recip(den, den)
                  T.tt(scl2, den, pg, ALU.mult)
                  T.ts(comb, ee, scl2, ALU.mult)
                  psT = P[2 + t % 2]
                  T.tr(psT[0:32, 0:128], comb, CM["IDN"])
                  T.copy(combT[:, tl], psT[0:32, 0:128], eng="act")

              ck(7, [combT[:, :], lg, lm, ee, selm, comb, t8, small[:, :]])
              ybank = [0]
              pend = None

              def down(e_i, tc, hd, wd):
                  for i in range(4):
                      t = tc * 4 + i
                      for half in range(2):
                          ps = P[5 + ybank[0] % 3]
                          ybank[0] += 1
                          for fc in range(2):
                              T.mm(ps[:, :], hd[:, fc, i * 128:(i + 1) * 128], wd[:, fc, half * 512:(half + 1) * 512],
                                   start=(fc == 0), stop=(fc == 1))
                          ysl = yacc[:, t, half * 512:(half + 1) * 512]
                          T.tt(ysl, ysl, ps[:, :], ALU.add)

              it = 0
              for e_i in range(32):
                  bi = e_i % 2
                  T.dma(wstage[:, :], wg_d[e_i, :, :])
                  T.copy(wgb[bi][:, :, :], wstage[:, :].rearrange("p (c f) -> p c f", c=8), eng="pool")
                  T.dma(wstage[:, :], wu_d[e_i, :, :])
                  T.copy(wub[bi][:, :, :], wstage[:, :].rearrange("p (c f) -> p c f", c=8), eng="pool")
                  T.dma(wstage[:, :], wd_d[e_i, :, :])
                  T.copy(wdb[bi][:, :, :], wstage[:, :].rearrange("p (c d) -> p c d", c=2), eng="pool")
                  for tc in range(4):
                      cs = slice(tc * 512, (tc + 1) * 512)
                      for fc in range(2):
                          for c in range(8):
                              T.mm(P[fc][:, :], wgb[bi][:, c, fc * 128:(fc + 1) * 128], h2T[:, c, cs], start=(c == 0), stop=(c == 7))
                          for c in range(8):
                              T.mm(P[2 + fc][:, :], wub[bi][:, c, fc * 128:(fc + 1) * 128], h2T[:, c, cs], start=(c == 0), stop=(c == 7))
                      T.mm(P[4][:, :], selst[:, e_i, :], combT[:, cs])
                      if pend is not None:
                          down(*pend)
                      hd = hid[it % 2]
                      it += 1
                      for fc in range(2):
                          T.act(sg[:, fc, :], P[fc][:, :], AF.Silu)
                          T.tt(sg[:, fc, :], sg[:, fc, :], P[2 + fc][:, :], ALU.mult)
                          T.tt(hd[:, fc, :], sg[:, fc, :], P[4][:, :], ALU.mult)
                      pend = (e_i, tc, hd, wdb[bi])
              down(*pend)
              pend = None

              ck(8, [yacc[:, 0, :], yacc[:, 9, :]])
              fg = carve(mixT_t, 0, 4096, F32)
              T.dma(fg, fg_d[:, :])
              for t in range(NT):
                  ob = xt[t % 2]
                  rs = rms_rstd(yacc[:, t, :], D, xn[:, :])
                  T.stt(ob[:, :], yacc[:, t, :], rs, fg, ALU.mult, ALU.mult)
                  T.dma(out_d[s, t * 128:(t + 1) * 128, :], ob[:, :])

        except _Done:
            pass
        T.finish()
        T.emit()
    return nc


_NC = None


def _prep_shared(inp):
    f = np.float32
    w_in = np.ascontiguousarray(inp["w_in"][0].reshape(8, 128, 3592).transpose(1, 0, 2), dtype=f)
    w_out = np.ascontiguousarray(inp["w_out"][0].reshape(8, 128, 1024).transpose(1, 0, 2), dtype=f)
    wr = np.concatenate([inp["w_group"][0], inp["w_router"][0]], axis=1)
    w_r = np.ascontiguousarray(wr.reshape(8, 128, 36).transpose(1, 0, 2), dtype=f)
    wg = np.ascontiguousarray(inp["w_gate"][0].reshape(32, 8, 128, 256).transpose(0, 2, 1, 3).reshape(32, 128, 2048), dtype=f)
    wu = np.ascontiguousarray(inp["w_up"][0].reshape(32, 8, 128, 256).transpose(0, 2, 1, 3).reshape(32, 128, 2048), dtype=f)
    wd = np.ascontiguousarray(inp["w_down"][0].reshape(32, 2, 128, 1024).transpose(0, 2, 1, 3).reshape(32, 128, 2048), dtype=f)
    gains = np.concatenate([inp["attn_norm"][0].reshape(8, 128).T, inp["ffn_norm"][0].reshape(8, 128).T], axis=1)
    fg = np.broadcast_to(inp["final_norm"][None, :], (128, D))
    gn4 = np.broadcast_to(np.tile(inp["gdn_norm"][0], 4)[None, :], (128, 512))
    cwv = inp["conv_w"][0].reshape(4, 12, 128).transpose(2, 1, 0).reshape(128, 48)
    dtb = np.broadcast_to(np.tile(inp["dt_bias"][0], 16)[None, :], (128, 64))
    alog = np.broadcast_to(np.tile(inp["A_log"][0], 16)[None, :], (128, 64))
    kind = (np.arange(S)[None, :] // 256 == np.arange(8)[:, None])
    sele = np.zeros((32, 32, 128), f)
    for e in range(32):
        sele[e, e, :] = 1.0
    c = lambda a: np.ascontiguousarray(a, dtype=f)
    return {"w_in": w_in, "w_out": w_out, "w_r": w_r, "w_gate": wg, "w_up": wu, "w_down": wd,
            "cm": _const_mats(), "ac": _attn_consts(), "kind": c(kind), "gains": c(gains), "fgain": c(fg),
            "gn4": c(gn4), "cw": c(cwv), "dtb": c(dtb), "alog": c(alog), "sele": c(sele.reshape(32, 4096))}


def kernel(**inputs):
    global _NC
    inp = {k: np.asarray(v) for k, v in inputs.items()}
    if _NC is None:
        _NC = build_program()
    shared = _prep_shared(inp)
    x = np.ascontiguousarray(inp["x"], dtype=np.float32)
    in_maps = []
    for c in range(8):
        m = dict(shared)
        m["x"] = np.ascontiguousarray(x[c * NSEQ:(c + 1) * NSEQ])
        in_maps.append(m)
    res = run_bass_kernel_spmd(_NC, in_maps, core_ids=list(range(8)))
    out = np.concatenate([np.asarray(r["out"]) for r in res.results], axis=0)
    return out.astype(np.float32, copy=False)
```

```python
import numpy as np
import concourse.bass as bass
import concourse.mybir as mybir
from concourse.bass_utils import run_bass_kernel_spmd

F32 = mybir.dt.float32
BF16 = mybir.dt.bfloat16
AF = mybir.ActivationFunctionType
ALU = mybir.AluOpType
AX = mybir.AxisListType

D = 1024
S = 2048
NT = S // 128
NSEQ = 2
EPS = 1e-6
BIG = 30000.0
_ESZ = {F32: 4, BF16: 2}


def _esz(dt):
    return _ESZ.get(dt, 4)


class Trk:
    def __init__(self, nc, sems, dsems):
        self.nc = nc
        self.sem = sems
        self.dsem = dsems
        self.cnt = {e: 0 for e in ("pe", "act", "dve", "pool")}
        self.duse = [0] * len(dsems)
        self.dnext = 0
        self.ops = {e: [] for e in ("pe", "act", "dve", "pool", "sp")}
        self.waited = {e: {} for e in self.ops}
        self.acc = {}
        self.dma_tokens = []

    @staticmethod
    def box(ap):
        t = ap.tensor
        dims = ap.ap
        es = _esz(ap.dtype)
        if "DRAM" in str(ap.space):
            ext = 1
            for st, c in dims:
                ext += (c - 1) * abs(st)
            return (t.name, 0, 1, ap.offset * es, (ap.offset + ext) * es)
        p0 = ap.start_partition()
        p1 = p0 + ap.partition_size()
        row = dims[0][0]
        f0 = ap.offset - p0 * row if row > 0 else ap.offset
        ext = 1
        for st, c in dims[1:]:
            ext += (c - 1) * abs(st)
        if "PSUM" in str(ap.space):
            return (t.name, (p0 // 32) * 32, ((p1 + 31) // 32) * 32, 0, 2048)
        return (t.name, p0, p1, f0 * es, (f0 + ext) * es)

    @staticmethod
    def _ov(a, b):
        return a[1] < b[2] and b[1] < a[2] and a[3] < b[4] and b[3] < a[4]

    @staticmethod
    def _contains(a, b):
        return a[1] <= b[1] and b[2] <= a[2] and a[3] <= b[3] and b[4] <= a[4]

    def _track(self, engine, outs, ins, token):
        deps = set()
        outs = list(outs) + [a for a in ins if "PSUM" in str(a.space)]
        ins = [a for a in ins if "PSUM" not in str(a.space)]
        rb = [self.box(a) for a in ins]
        wb = [self.box(a) for a in outs]
        for b in rb:
            for r in self.acc.get(b[0], ()):
                if r[2] and self._ov(r[0], b):
                    deps.add(r[1])
        for b in wb:
            for r in self.acc.get(b[0], ()):
                if self._ov(r[0], b):
                    deps.add(r[1])
        for b in wb:
            lst = self.acc.setdefault(b[0], [])
            lst[:] = [r for r in lst if not self._contains(b, r[0])]
            lst.append((b, token, True, engine))
        for b in rb:
            lst = self.acc.setdefault(b[0], [])
            rep = False
            if engine != "dma":
                for i, r in enumerate(lst):
                    if (not r[2]) and r[3] == engine and r[0] == b:
                        lst[i] = (b, token, False, engine)
                        rep = True
                        break
            if not rep:
                lst.append((b, token, False, engine))
        deps.discard(token)
        return deps

    def _waits(self, engine, deps):
        out = []
        w = self.waited[engine]
        best = {}
        for key, val in deps:
            if best.get(key, 0) < val:
                best[key] = val
        for key, val in sorted(best.items(), key=lambda d: str(d[0])):
            if key == "pe" and engine == "pe":
                continue
            if w.get(key, 0) >= val:
                continue
            w[key] = val
            sem = self.sem[key] if isinstance(key, str) else self.dsem[key[1]]
            out.append((sem, val))
        return out

    def op(self, engine, fn, outs, ins, signal=True):
        if signal:
            self.cnt[engine] += 1
            token = (engine, self.cnt[engine])
        else:
            token = (engine, self.cnt[engine] + 1)
        deps = self._track(engine, outs, ins, token)
        self.ops[engine].append((self._waits(engine, deps), fn, (self.sem[engine], 1) if signal else None))

    def dma(self, out, in_, queue="sp", **kw):
        k = self.dnext
        self.dnext = (self.dnext + 1) % len(self.dsem)
        deps = set()
        if self.duse[k] > 0:
            deps.add((("d", k), 16 * self.duse[k]))
        self.duse[k] += 1
        token = (("d", k), 16 * self.duse[k])
        deps |= self._track("dma", [out], [in_], token)
        self.ops[queue].append((self._waits(queue, deps),
                                lambda e, o=out, i=in_, kw=kw: e.dma_start(out=o, in_=i, **kw),
                                (self.dsem[k], 16)))

    def finish(self):
        deps = set()
        for k, u in enumerate(self.duse):
            if u > 0:
                deps.add((("d", k), 16 * u))
        for e in ("pe", "act", "dve", "pool"):
            if self.cnt[e] > 0:
                deps.add((e, self.cnt[e]))
        w = self._waits("sp", deps)
        self.ops["sp"].append((w, None, None))

    def emit(self):
        with self.nc.Block() as block:
            for name, deco in (("sp", block.sync), ("act", block.scalar), ("dve", block.vector),
                               ("pool", block.gpsimd), ("pe", block.tensor)):
                ops = self.ops[name]

                def body(e, ops=ops):
                    for waits, fn, inc in ops:
                        for sem, val in waits:
                            e.wait_ge(sem, val)
                        if fn is None:
                            continue
                        ins = fn(e)
                        if inc is not None:
                            ins.then_inc(inc[0], inc[1])
                deco(body)

    def mm(self, out, lhsT, rhs, start=True, stop=True):
        self.op("pe", lambda e: e.matmul(out, lhsT, rhs, start=start, stop=stop), [out], [lhsT, rhs], signal=stop)

    def tr(self, out, in_, ident):
        self.op("pe", lambda e: e.transpose(out, in_, ident), [out], [in_, ident])

    def act(self, out, in_, func, bias=None, scale=None, accum_out=None):
        kw = {}
        ins = [in_]
        outs = [out]
        if bias is not None:
            kw["bias"] = bias
            if not isinstance(bias, (int, float)):
                ins.append(bias)
        if scale is not None:
            kw["scale"] = scale
            if not isinstance(scale, (int, float)):
                ins.append(scale)
        if accum_out is not None:
            kw["accum_out"] = accum_out
            outs.append(accum_out)
        self.op("act", lambda e: e.activation(out=out, in_=in_, func=func, **kw), outs, ins)

    def ts(self, out, in0, s1, op0, s2=None, op1=None, eng="dve"):
        ins = [in0] + [s for s in (s1, s2) if s is not None and not isinstance(s, (int, float))]
        kw = {}
        if op1 is not None:
            kw["op1"] = op1
        self.op(eng, lambda e: e.tensor_scalar(out=out, in0=in0, scalar1=s1, scalar2=s2, op0=op0, **kw), [out], ins)

    def tt(self, out, in0, in1, op, eng="dve"):
        self.op(eng, lambda e: e.tensor_tensor(out=out, in0=in0, in1=in1, op=op), [out], [in0, in1])

    def stt(self, out, in0, scalar, in1, op0, op1):
        ins = [in0, in1] + ([] if isinstance(scalar, (int, float)) else [scalar])
        self.op("dve", lambda e: e.scalar_tensor_tensor(out=out, in0=in0, scalar=scalar, in1=in1, op0=op0, op1=op1),
                [out], ins)

    def copy(self, out, in_, eng="dve"):
        if eng == "act":
            self.act(out, in_, AF.Copy)
        else:
            self.op(eng, lambda e: e.tensor_copy(out=out, in_=in_), [out], [in_])

    def recip(self, out, in_):
        self.op("dve", lambda e: e.reciprocal(out=out, in_=in_), [out], [in_])

    def red(self, out, in_, op, axis=None):
        ax = AX.X if axis is None else axis
        self.op("dve", lambda e: e.tensor_reduce(out=out, in_=in_, axis=ax, op=op), [out], [in_])

    def max8(self, out, in_):
        self.op("dve", lambda e: e.max(out=out, in_=in_), [out], [in_])

    def memset(self, ap, val, eng="dve"):
        self.op(eng, lambda e: e.memset(ap, val), [ap], [])


CM_NAMES = ["IDN", "ONES", "LTU", "MUS", "MLS", "CA", "CB", "SC", "TRI"]


def _const_mats():
    i = np.arange(128)
    same = (i[:, None] // 64) == (i[None, :] // 64)
    m = {}
    m["IDN"] = np.eye(128)
    m["ONES"] = np.ones((128, 128))
    m["LTU"] = (same & (i[:, None] <= i[None, :]))
    m["MUS"] = (same & (i[:, None] < i[None, :]))
    m["MLS"] = (same & (i[:, None] > i[None, :]))
    m["CA"] = np.broadcast_to((i[:, None] < 64), (128, 128))
    m["CB"] = np.broadcast_to((i[:, None] >= 64), (128, 128))
    m["SC"] = same
    m["TRI"] = (i[:, None] <= i[None, :])
    return np.concatenate([np.asarray(m[n], dtype=np.float32) for n in CM_NAMES], axis=1)


def _attn_consts():
    neg = np.zeros((4, 8, 8), np.float32)
    cst = np.zeros((8, 8, 8), np.float32)
    base = np.zeros((8, 8, 8), np.float32)
    j = np.arange(8)
    for own in range(8):
        cst[own] = np.where(j == own, 0.0, -BIG)[None, :]
        base[own] = np.where(j <= own, 0.0, -BIG)[None, :]
        if own >= 4:
            neg[own - 4] = np.where(j < own, 0.0, -1e30)[None, :]
    row = np.concatenate([neg.reshape(-1), cst.reshape(-1), base.reshape(-1)])
    return np.broadcast_to(row[None, :], (128, row.size)).astype(np.float32).copy()


class _Done(Exception):
    pass


def build_program(stage=0, nseq=NSEQ):
    nc = bass.Bass("TRN2", target_bir_lowering=False)
    dbg_d = nc.dram_tensor("dbg", [16, 128, 2048], F32, kind="ExternalOutput").ap() if stage else None

    def din(name, shape):
        return nc.dram_tensor(name, list(shape), F32, kind="ExternalInput").ap()

    x_d = din("x", [NSEQ, S, D])
    win_d = din("w_in", [128, 8, 3592])
    wout_d = din("w_out", [128, 8, 1024])
    wr_d = din("w_r", [128, 8, 36])
    ne_decl = 32 if stage in (0, 6, 7, 8) else 1
    wg_d = din("w_gate", [ne_decl, 128, 8 * 256])
    wu_d = din("w_up", [ne_decl, 128, 8 * 256])
    wd_d = din("w_down", [ne_decl, 128, 2 * 1024])
    cm_d = din("cm", [128, 9 * 128])
    ac_d = din("ac", [128, 256 + 512 + 512])
    kind_d = din("kind", [8, S])
    gains_d = din("gains", [128, 16])
    fg_d = din("fgain", [128, D])
    gn_d = din("gn4", [128, 512])
    cw_d = din("cw", [128, 48])
    dtb_d = din("dtb", [128, 64])
    alog_d = din("alog", [128, 64])
    sele_d = din("sele", [32, 32 * 128])
    out_d = nc.dram_tensor("out", [NSEQ, S, D], F32, kind="ExternalOutput").ap()

    import contextlib
    es = contextlib.ExitStack()
    with es:
        def sb(name, shape, dt=F32):
            return es.enter_context(nc.sbuf_tensor("sb_" + name, list(shape), dt))

        def sem(name):
            return es.enter_context(nc.semaphore(name))

        sems = {e: sem("s_" + e) for e in ("pe", "act", "dve", "pool")}
        dsems = [sem("d%d" % i) for i in range(24)]
        T = Trk(nc, sems, dsems)

        cm = sb("cm", [128, 9 * 128])
        CM = {n: cm[:, i * 128:(i + 1) * 128] for i, n in enumerate(CM_NAMES)}
        ac = sb("ac", [128, 1280])
        negmask = ac[:, 0:256].rearrange("p (o c) -> p o c", o=4)
        cstb = ac[:, 256:768].rearrange("p (o c) -> p o c", o=8)
        basebias = ac[:, 768:1280].rearrange("p (o c) -> p o c", o=8)
        idb = sb("idb", [128, 128], BF16)
        trib = sb("trib", [128, 128], BF16)
        idn4 = sb("idn4", [128, 4, 128])
        ltu4 = sb("ltu4", [128, 4, 128])
        gains = sb("gains", [128, 16])
        gn4 = sb("gn4", [128, 512])
        cw = sb("cw", [128, 48])
        dtb = sb("dtb", [128, 64])
        nega = sb("nega", [128, 64])
        gsm = sb("gsm", [128, 96])
        small = sb("small", [128, 256])
        xt = [sb("xt%d" % i, [128, D]) for i in range(2)]
        xn = sb("xn", [128, D], BF16)
        wst = [sb("wst%d" % i, [128, 8, 128]) for i in range(2)]
        wbf = [sb("wbf%d" % i, [128, 8, 128], BF16) for i in range(2)]
        hT = sb("hT", [128, 8 * S], BF16)
        mixT_t = sb("mixT", [128, 8 * S], BF16)
        X = sb("X", [128, 22528])
        P = [es.enter_context(nc.psum_tensor("ps%d" % i, [128, 512], F32)) for i in range(8)]

        hT3 = hT[:, :].rearrange("p (c t) -> p c t", c=8)
        mixT = mixT_t[:, :].rearrange("p (c t) -> p c t", c=8)

        def carve(arena, off_b, nbytes, dt, parts=128):
            aes = _esz(arena.dtype)
            v = arena[0:parts, off_b // aes:(off_b + nbytes) // aes]
            if dt != arena.dtype:
                v = v.bitcast(dt)
            return v

        def pbf(bank):
            return P[bank][:, :].bitcast(BF16)

        sc_i = [0]

        def sc(n):
            if sc_i[0] + n > 256:
                sc_i[0] = 0
            a = small[:, sc_i[0]:sc_i[0] + n]
            sc_i[0] += n
            return a

        T.dma(cm[:, :], cm_d[:, :])
        T.dma(ac[:, :], ac_d[:, :])
        T.dma(gains[:, :], gains_d[:, :])
        T.dma(gn4[:, :], gn_d[:, :])
        T.dma(cw[:, :], cw_d[:, :])
        T.dma(dtb[:, :], dtb_d[:, :])
        T.dma(nega[:, :], alog_d[:, :])
        T.copy(idb[:, :], CM["IDN"])
        T.copy(trib[:, :], CM["TRI"])
        for h in range(4):
            T.copy(idn4[:, h, :], CM["IDN"])
            T.copy(ltu4[:, h, :], CM["LTU"])
        T.act(nega[:, :], nega[:, :], AF.Exp)
        T.ts(nega[:, :], nega[:, :], -1.0, ALU.mult)

        wload_i = [0]

        def load_w(src_ap, ncols):
            i = wload_i[0] % 2
            wload_i[0] += 1
            T.dma(wst[i][:, :, 0:ncols], src_ap)
            T.copy(wbf[i][:, :, 0:ncols], wst[i][:, :, 0:ncols], eng="pool")
            return wbf[i]

        def rms_rstd(src, n, junk):
            ss = sc(1)
            T.act(junk, src, AF.Square, accum_out=ss)
            sd = sc(1)
            T.act(sd, ss, AF.Sqrt, bias=epsc[:, 0:1], scale=1.0 / n)
            rs = sc(1)
            T.recip(rs, sd)
            return rs

        epsc = sb("epsc", [128, 2])
        T.memset(epsc[:, 0:1], EPS)
        T.memset(epsc[:, 1:2], 1.0)

        def norm_T(src, gcol0, dst3, tok0):
            rs = rms_rstd(src, D, xn[:, :])
            T.ts(xn[:, :], src, rs, ALU.mult)
            pt = pbf(7).rearrange("p (c t) -> p c t", c=8)
            for c in range(8):
                T.tr(pt[:, c, :], xn[:, c * 128:(c + 1) * 128], idb[:, :])
            for c in range(8):
                T.act(dst3[:, c, tok0:tok0 + 128], pt[:, c, :], AF.Copy, scale=gains[:, gcol0 + c:gcol0 + c + 1])

        dbgn = [0]
        dbgst = sb("dbgst", [128, 2048]) if stage else None

        def ck(k, aps):
            if stage != k:
                return
            for a in aps:
                n = a.shape[-1]
                p = a.shape[0]
                T.copy(dbgst[0:p, 0:n], a)
                T.dma(dbg_d[dbgn[0], 0:p, 0:n], dbgst[0:p, 0:n])
                dbgn[0] += 1
            raise _Done()

        try:
          for s in range(nseq):
              for t in range(NT):
                  xb = xt[t % 2]
                  T.dma(xb[:, :], x_d[s, t * 128:(t + 1) * 128, :])
                  norm_T(xb[:, :], 0, hT3, t * 128)

              ck(1, [hT3[:, 0, :], hT3[:, 7, :]])
              qa = carve(X, 0, 32768, BF16, parts=72).rearrange("p (h t) -> p h t", h=8)
              ka = carve(X, 32768, 32768, BF16, parts=72).rearrange("p (h t) -> p h t", h=8)
              vaug = carve(X, 65536, 16 * 8 * 65 * 2, BF16).rearrange("p (t h e) -> p t h e", t=16, h=8)
              o2 = 65536 + 16 * 8 * 65 * 2
              ya = [carve(X, o2 + i * 1024, 1024, BF16) for i in range(4)]
              o2 += 4096
              pTb = [carve(X, o2 + i * 1024, 1024, BF16) for i in range(2)]
              o2 += 2048
              kms = carve(X, o2, 256, F32, parts=64).rearrange("p (h j) -> p h j", h=8)
              o2 += 256
              kmb = carve(X, o2, 128, BF16, parts=64).rearrange("p (h j) -> p h j", h=8)
              o2 += 128
              gm = carve(X, o2, 256, F32)
              o2 += 256
              top = carve(X, o2, 256, F32).rearrange("p (h j) -> p h j", h=8)
              o2 += 256
              selb = carve(X, o2, 256, F32)
              o2 += 256
              sbias = carve(X, o2, 128, BF16)
              o2 += 128
              stg = carve(X, o2, 256, BF16, parts=64)
              o2 += 256

              T.memset(vaug[:, :, :, 64:65], 1.0)
              for half in range(2):
                  T.dma(xt[0][64:72, :], kind_d[:, half * 1024:(half + 1) * 1024])
                  for h in range(8):
                      T.copy(ka[64:72, h, half * 1024:(half + 1) * 1024], xt[0][64:72, :], eng="pool")

              for which, dst in ((0, qa), (1, ka)):
                  for g2 in range(4):
                      c0 = which * 512 + g2 * 128
                      w = load_w(win_d[:, :, c0:c0 + 128], 128)
                      for hh in range(2):
                          h = g2 * 2 + hh
                          for tc in range(4):
                              ps = P[(h * 4 + tc) % 2]
                              for c in range(8):
                                  T.mm(ps[0:64, :], w[:, c, hh * 64:(hh + 1) * 64], hT3[:, c, tc * 512:(tc + 1) * 512],
                                       start=(c == 0), stop=(c == 7))
                              T.copy(dst[0:64, h, tc * 512:(tc + 1) * 512], ps[0:64, :], eng=("act" if tc % 2 else "dve"))
              for g2 in range(4):
                  c0 = 1024 + g2 * 128
                  w = load_w(win_d[:, :, c0:c0 + 128], 128)
                  for t in range(NT):
                      ps = P[t % 2]
                      for c in range(8):
                          T.mm(ps[:, 0:128], hT3[:, c, t * 128:(t + 1) * 128], w[:, c, :], start=(c == 0), stop=(c == 7))
                      T.copy(vaug[:, t, g2 * 2:g2 * 2 + 2, 0:64], ps[:, 0:128].rearrange("p (h e) -> p h e", h=2),
                             eng=("act" if t % 2 else "dve"))
              for h in range(8):
                  T.red(kms[:, h, :], ka[0:64, h, :].rearrange("p (j l) -> p j l", j=8), ALU.add)
              T.ts(kmb[:, :, :], kms[:, :, :], 1.0 / 256.0, ALU.mult)
              for qt in range(NT):
                  own = qt // 2
                  if own >= 4:
                      psg = P[6]
                      for h in range(8):
                          T.mm(psg[:, h * 8:(h + 1) * 8], qa[0:64, h, qt * 128:(qt + 1) * 128], kmb[:, h, :])
                      T.tt(gm, psg[:, 0:64], negmask[:, own - 4, :], ALU.add)
                      gm3 = gm.rearrange("p (h j) -> p h j", h=8)
                      for h in range(8):
                          T.max8(top[:, h, :], gm3[:, h, :])
                      for h in range(8):
                          T.ts(selb[:, h * 8:(h + 1) * 8], gm3[:, h, :], top[:, h, 2:3], ALU.is_ge)
                      T.stt(sbias, selb, BIG, cstb[:, own, :], ALU.mult, ALU.add)
                  else:
                      T.copy(sbias, basebias[:, own, :])
                  pt = pbf(7)
                  T.tr(pt[0:64, 0:128], sbias, idb[:, :])
                  T.copy(stg, pt[0:64, 0:128])
                  for h in range(8):
                      T.dma(qa[64:72, h, qt * 128:(qt + 1) * 128], stg[h * 8:(h + 1) * 8, :])
              ck(2, [qa[:, 0, :], qa[:, 5, :], ka[:, 0, :], ka[:, 5, :], vaug[:, 3, :, :].rearrange('p h e -> p (h e)'), kmb[:, :, :].rearrange('p h j -> p (h j)')])
              for qc in range(4):
                  for h in range(8):
                      nkt = 4 * qc + 4

                      def score(kt, qc=qc, h=h):
                          i0 = max(0, kt - 4 * qc)
                          ncol = (4 - i0) * 128
                          c0 = qc * 512 + i0 * 128
                          ps = P[kt % 2]
                          T.mm(ps[:, 0:ncol], ka[0:72, h, kt * 128:(kt + 1) * 128], qa[0:72, h, c0:c0 + ncol])
                          pT = pTb[kt % 2]
                          T.act(pT[:, 0:ncol], ps[:, 0:ncol], AF.Exp, scale=0.125)
                          if kt >= 4 * qc:
                              T.tt(pT[:, 0:128], pT[:, 0:128], trib[:, :], ALU.mult, eng="pool")

                      score(0)
                      for kt in range(nkt):
                          if kt + 1 < nkt:
                              score(kt + 1)
                          i0 = max(0, kt - 4 * qc)
                          pT = pTb[kt % 2]
                          for i in range(i0, 4):
                              qt = 4 * qc + i
                              T.mm(P[2 + i][:, 0:65], pT[:, (i - i0) * 128:(i - i0 + 1) * 128], vaug[:, kt, h, :],
                                   start=(kt == 0), stop=(kt == qt))
                      for i in range(4):
                          rec = sc(1)
                          T.recip(rec, P[2 + i][:, 64:65])
                          T.act(ya[i][:, h * 64:(h + 1) * 64], P[2 + i][:, 0:64], AF.Copy, scale=rec)
                  for i in range(4):
                      qt = 4 * qc + i
                      pt = pbf(7).rearrange("p (c t) -> p c t", c=8)
                      for c in range(4):
                          T.tr(pt[:, c, :], ya[i][:, c * 128:(c + 1) * 128], idb[:, :])
                      T.copy(mixT[:, 0:4, qt * 128:(qt + 1) * 128], pt[:, 0:4, :])

              ck(3, [mixT[:, 0, :], mixT[:, 3, :]])
              qnT = carve(X, 0, 16384, BF16).rearrange("p (h t) -> p h t", h=4)
              knT = carve(X, 16384, 16384, BF16).rearrange("p (h t) -> p h t", h=4)
              vT = carve(X, 32768, 16384, BF16).rearrange("p (h t) -> p h t", h=4)
              zs = carve(X, 49152, 16384, BF16).rearrange("p (t e) -> p t e", t=16)
              o2 = 65536
              pre = carve(X, o2, 2052 * 4, F32)
              o2 += 2052 * 4
              cacc = carve(X, o2, 8192, F32)
              o2 += 8192
              tsq = carve(X, o2, 2048, F32)
              o2 += 2048
              tsd = carve(X, o2, 2048, F32)
              o2 += 2048
              gt = carve(X, o2, 256, F32).rearrange("p (t h) -> p t h", t=16)
              o2 += 256
              bet = carve(X, o2, 256, F32).rearrange("p (t h) -> p t h", t=16)
              o2 += 256
              xab = carve(X, o2, 256, F32)
              o2 += 256
              T.memset(pre[:, 0:3], 0.0)

              for ch in range(12):
                  c0 = 1536 + ch * 128
                  w = load_w(win_d[:, :, c0:c0 + 128], 128)
                  for tc in range(4):
                      ps = P[tc % 2]
                      for c in range(8):
                          T.mm(ps[:, :], w[:, c, :], hT3[:, c, tc * 512:(tc + 1) * 512], start=(c == 0), stop=(c == 7))
                      T.copy(pre[:, 3 + tc * 512:3 + (tc + 1) * 512], ps[:, :], eng="act")
                  T.ts(cacc, pre[:, 0:S], cw[:, ch * 4:ch * 4 + 1], ALU.mult)
                  for i in range(1, 4):
                      T.stt(cacc, pre[:, i:i + S], cw[:, ch * 4 + i:ch * 4 + i + 1], cacc, ALU.mult, ALU.add)
                  kind, h = ch // 4, ch % 4
                  if kind == 2:
                      T.act(vT[:, h, :], cacc, AF.Silu)
                  else:
                      T.act(cacc, cacc, AF.Silu)
                      dst = qnT if kind == 0 else knT
                      scl = (128.0 ** -0.5) if kind == 0 else 1.0
                      for tc in range(4):
                          sl = slice(tc * 512, (tc + 1) * 512)
                          T.act(tsq, cacc[:, sl], AF.Square)
                          psn = P[2 + tc % 2]
                          T.mm(psn[:, :], CM["ONES"], tsq)
                          T.act(tsd, psn[:, :], AF.Sqrt, bias=epsc[:, 0:1], scale=1.0)
                          T.recip(tsd, tsd)
                          T.stt(dst[:, h, sl], cacc[:, sl], scl, tsd, ALU.mult, ALU.mult)
              for g2 in range(4):
                  c0 = 3072 + g2 * 128
                  w = load_w(win_d[:, :, c0:c0 + 128], 128)
                  for t in range(NT):
                      ps = P[t % 2]
                      for c in range(8):
                          T.mm(ps[:, 0:128], hT3[:, c, t * 128:(t + 1) * 128], w[:, c, :], start=(c == 0), stop=(c == 7))
                      T.act(zs[:, t, g2 * 128:(g2 + 1) * 128], ps[:, 0:128], AF.Silu)
              w = load_w(win_d[:, :, 3584:3592], 8)
              psab = P[4]
              for t in range(NT):
                  for c in range(8):
                      T.mm(psab[:, t * 8:(t + 1) * 8], hT3[:, c, t * 128:(t + 1) * 128], w[:, c, 0:8],
                           start=(c == 0), stop=(c == 7))
              pab3 = psab[:, 0:128].rearrange("p (t e) -> p t e", t=16)
              xab3 = xab.rearrange("p (t h) -> p t h", t=16)
              T.tt(xab3, pab3[:, :, 0:4], dtb[:, :].rearrange("p (t h) -> p t h", t=16), ALU.add)
              T.act(xab, xab, AF.Exp)
              T.act(xab, xab, AF.Ln, bias=epsc[:, 1:2], scale=1.0)
              T.tt(gt, xab3, nega[:, :].rearrange("p (t h) -> p t h", t=16), ALU.mult)
              T.act(bet, pab3[:, :, 4:8], AF.Sigmoid)

              ck(4, [qnT[:, 0, :], knT[:, 2, :], vT[:, 1, :], zs[:, 5, :], gt[:, :, :].rearrange('p t h -> p (t h)'), bet[:, :, :].rearrange('p t h -> p (t h)')])
              hoff = [0]

              def hc(nbytes, dt, shape4=True):
                  v = carve(hT, hoff[0], nbytes, dt)
                  hoff[0] += nbytes
                  return v.rearrange("p (h e) -> p h e", h=4) if shape4 else v

              gbc = hc(2048, F32)
              dt4 = hc(2048, F32)
              dl4 = hc(2048, F32)
              A4 = hc(2048, F32)
              qkm4 = hc(1024, BF16)
              qb = [hc(1024, BF16) for _ in range(2)]
              qtb = [hc(1024, BF16) for _ in range(2)]
              pb = [hc(1024, BF16) for _ in range(2)]
              bgk = hc(1024, BF16)
              kdec = hc(1024, BF16)
              bv = hc(1024, BF16)
              uv = hc(2048, F32)
              wT = hc(1024, BF16)
              ub = hc(1024, BF16)
              o1s = hc(2048, F32)
              ot = hc(2048, F32)
              S32 = hc(2048, F32)
              Sb = hc(1024, BF16)
              zg = hc(2048, F32)
              yb = hc(1024, BF16)
              Gs = gsm[:, 0:16]
              ex = gsm[:, 16:48]
              assert hoff[0] <= 32768
              T.memset(S32[:, :, :], 0.0)
              T.memset(Sb[:, :, :], 0.0)
              gam, glab, kds, bg, d1 = ex[:, 0:4], ex[:, 4:12], ex[:, 12:16], ex[:, 16:20], ex[:, 20:24]
              ss4, rs4 = ex[:, 24:28], ex[:, 28:32]
              kdsA, kdsB = gsm[:, 48:52], gsm[:, 52:56]
              kdecB = dl4[:, :, :].rearrange('p h e -> p (h e)')[:, 0:256].bitcast(BF16).rearrange('p (h e) -> p h e', h=4)
              T.memset(ub[:, :, :], 0.0)

              def b4(bank):
                  return P[bank][:, :].rearrange("p (h e) -> p h e", h=4)

              def b4bf(bank):
                  return pbf(bank)[:, 0:512].rearrange("p (h e) -> p h e", h=4)

              for t in range(NT):
                  tl = slice(t * 128, (t + 1) * 128)
                  g_t, b_t = gt[:, t, :], bet[:, t, :]
                  psG = P[0]
                  for i, mname in enumerate(("LTU", "SC", "CA", "CB")):
                      T.mm(psG[:, i * 4:(i + 1) * 4], CM[mname], g_t)
                  T.copy(Gs, psG[:, 0:16])
                  ck(41, [Gs])
                  T.act(gam, Gs[:, 0:4], AF.Exp)
                  T.act(glab, Gs[:, 8:16], AF.Exp)
                  T.tt(d1, Gs[:, 4:8], Gs[:, 0:4], ALU.subtract)
                  T.act(kds, d1, AF.Exp)
                  T.tt(bg, b_t, gam, ALU.mult)
                  T.ts(kdsA, kds, CM["CA"][:, 0:1], ALU.mult)
                  T.ts(kdsB, kds, CM["CB"][:, 0:1], ALU.mult)
                  psR = b4(1)
                  for h in range(4):
                      T.ts(gbc[:, h, :], CM["ONES"], g_t[:, h:h + 1], ALU.mult)
                      T.mm(psR[:, h, :], gbc[:, h, :], CM["LTU"])
                  for h in range(4):
                      T.ts(dt4[:, h, :], psR[:, h, :], Gs[:, h:h + 1], ALU.subtract, 0.0, ALU.min)
                      T.ts(dl4[:, h, :], psR[:, h, :], Gs[:, h:h + 1], ALU.subtract, 0.0, ALU.max)
                  T.act(dt4[:, :, :], dt4[:, :, :], AF.Exp)
                  T.act(dl4[:, :, :], dl4[:, :, :], AF.Exp, scale=-1.0)
                  psK, psQ = b4(2), b4(3)
                  for h in range(4):
                      T.mm(psK[:, h, :], knT[:, h, tl], knT[:, h, tl])
                      T.mm(psQ[:, h, :], knT[:, h, tl], qnT[:, h, tl])
                  T.tt(A4[:, :, :], psK, dl4[:, :, :], ALU.mult)
                  for h in range(4):
                      T.stt(A4[:, h, :], A4[:, h, :], b_t[:, h:h + 1], CM["MLS"], ALU.mult, ALU.mult)
                  T.tt(dt4[:, :, :], psQ, dt4[:, :, :], ALU.mult)
                  T.tt(qkm4[:, :, :], dt4[:, :, :], ltu4[:, :, :], ALU.mult)
                  psN = b4(4)
                  for h in range(4):
                      T.tr(psN[:, h, :], A4[:, h, :], CM["IDN"])
                  T.copy(qtb[0][:, :, :], A4[:, :, :], eng="pool")
                  T.copy(qb[0][:, :, :], psN, eng="act")
                  T.tt(pb[0][:, :, :], idn4[:, :, :], psN, ALU.subtract)
                  cq, cp = 0, 0
                  for k in range(1, 6):
                      psA = b4(5)
                      for h in range(4):
                          T.mm(psA[:, h, :], qb[cq][:, h, :], qtb[cq][:, h, :])
                      T.copy(qtb[1 - cq][:, :, :], psA, eng="act")
                      if k < 5:
                          psB = b4(6)
                          for h in range(4):
                              T.mm(psB[:, h, :], qtb[cq][:, h, :], qb[cq][:, h, :])
                          T.copy(qb[1 - cq][:, :, :], psB)
                      psC = b4(0)
                      for h in range(4):
                          T.mm(psC[:, h, :], qtb[1 - cq][:, h, :], pb[cp][:, h, :])
                      T.tt(pb[1 - cp][:, :, :], pb[cp][:, :, :], psC, ALU.add)
                      cq, cp = 1 - cq, 1 - cp
                  TT = pb[cp]
                  ck(42, [TT[:, 0, :], TT[:, 3, :], qkm4[:, 1, :]])
                  ptk = pbf(7)[:, 0:512].rearrange('p (h e) -> p h e', h=4)
                  ptv = pbf(7)[:, 512:1024].rearrange('p (h e) -> p h e', h=4)
                  for h in range(4):
                      T.tr(ptk[:, h, :], knT[:, h, tl], idb[:, :])
                      T.tr(ptv[:, h, :], vT[:, h, tl], idb[:, :])
                  ck(46, [TT[:, 0, :]])
                  for h in range(4):
                      T.act(bgk[:, h, :], ptk[:, h, :], AF.Copy, scale=bg[:, h:h + 1])
                      T.ts(kdec[:, h, :], ptk[:, h, :], kdsA[:, h:h + 1], ALU.mult)
                      T.ts(kdecB[:, h, :], ptk[:, h, :], kdsB[:, h:h + 1], ALU.mult)
                      T.act(bv[:, h, :], ptv[:, h, :], AF.Copy, scale=b_t[:, h:h + 1])
                  ck(45, [bgk[:, 2, :], kdec[:, 0, :], kdecB[:, 3, :], bv[:, 1, :]])
                  psU, psW = b4(3), b4(4)
                  for h in range(4):
                      T.mm(psU[:, h, :], TT[:, h, :], bv[:, h, :])
                      T.mm(psW[:, h, :], bgk[:, h, :], TT[:, h, :])
                  T.copy(uv[:, :, :], psU)
                  T.copy(wT[:, :, :], psW, eng="act")
                  ck(44, [uv[:, 0, :], wT[:, 1, :], bgk[:, 2, :], kdecB[:, 3, :]])
                  for xi in range(2):
                      r = slice(xi * 64, (xi + 1) * 64)
                      psWS, psO1, psO2, psS = b4(5), b4(6), b4(0), b4(1)
                      kd = kdec if xi == 0 else kdecB
                      for h in range(4):
                          T.mm(psWS[:, h, :], wT[:, h, :], Sb[:, h, :])
                      T.tt(ub[r, :, :], uv[r, :, :], psWS[r, :, :], ALU.subtract)
                      for h in range(4):
                          T.mm(psO1[:, h, :], qnT[:, h, tl], Sb[:, h, :])
                          T.mm(psO2[:, h, :], qkm4[:, h, :], ub[:, h, :])
                          T.mm(psS[:, h, :], kd[:, h, :], ub[:, h, :])
                      for h in range(4):
                          T.act(o1s[r, h, :], psO1[r, h, :], AF.Copy, scale=gam[r, h:h + 1])
                      T.tt(ot[r, :, :], o1s[r, :, :], psO2[r, :, :], ALU.add)
                      for h in range(4):
                          T.stt(S32[:, h, :], S32[:, h, :], glab[:, xi * 4 + h:xi * 4 + h + 1], psS[:, h, :],
                                ALU.mult, ALU.add)
                      T.copy(Sb[:, :, :], S32[:, :, :], eng="pool")
                  ck(43, [ot[:, 0, :], ot[:, 3, :], S32[:, 2, :]])
                  for h in range(4):
                      T.act(o1s[:, h, :], ot[:, h, :], AF.Square, accum_out=ss4[:, h:h + 1])
                  T.act(rs4, ss4, AF.Sqrt, bias=epsc[:, 0:1], scale=1.0 / 128.0)
                  T.recip(rs4, rs4)
                  T.tt(zg[:, :, :], zs[:, t, :].rearrange("p (h e) -> p h e", h=4),
                       gn4[:, :].rearrange("p (h e) -> p h e", h=4), ALU.mult)
                  for h in range(4):
                      T.stt(yb[:, h, :], ot[:, h, :], rs4[:, h:h + 1], zg[:, h, :], ALU.mult, ALU.mult)
                  pty = b4bf(7)
                  for h in range(4):
                      T.tr(pty[:, h, :], yb[:, h, :], idb[:, :])
                  T.copy(mixT[:, 4:8, tl], pty)

              ck(5, [mixT[:, 4, :], mixT[:, 7, :]])
              yacc = carve(X, 0, 65536, F32).rearrange("p (t d) -> p t d", t=16)
              wo = carve(X, 65536, 16384, BF16).rearrange("p (c d) -> p c d", c=8)
              for g2 in range(8):
                  w = load_w(wout_d[:, :, g2 * 128:(g2 + 1) * 128], 128)
                  T.copy(wo[:, :, g2 * 128:(g2 + 1) * 128], w[:, :, :], eng="pool")
              for t in range(NT):
                  tl = slice(t * 128, (t + 1) * 128)
                  xb = xt[t % 2]
                  T.dma(xb[:, :], x_d[s, tl, :])
                  for half in range(2):
                      ps = P[half + 2 * (t % 2)]
                      for c in range(8):
                          T.mm(ps[:, :], mixT[:, c, tl], wo[:, c, half * 512:(half + 1) * 512], start=(c == 0), stop=(c == 7))
                      T.tt(yacc[:, t, half * 512:(half + 1) * 512], xb[:, half * 512:(half + 1) * 512], ps[:, :], ALU.add)
                  norm_T(yacc[:, t, :], 8, hT3, t * 128)

              ck(6, [yacc[:, 0, :], yacc[:, 9, :], hT3[:, 2, :]])
              h2T = hT3
              o2 = 65536
              wr = carve(X, o2, 8 * 36 * 2, BF16).rearrange("p (c e) -> p c e", c=8)
              o2 += 8 * 36 * 2
              combT = carve(X, o2, 4096, BF16, parts=32)
              o2 += 4096
              lg = carve(X, o2, 144, F32)
              o2 += 144
              lm = carve(X, o2, 128, F32)
              o2 += 128
              ee = carve(X, o2, 128, F32)
              o2 += 128
              selm = carve(X, o2, 128, F32)
              o2 += 128
              comb = carve(X, o2, 128, F32)
              o2 += 128
              t8 = carve(X, o2, 32, F32)
              o2 += 32
              sg = carve(X, o2, 4096, F32).rearrange("p (f t) -> p f t", f=2)
              o2 += 4096
              hid = [carve(X, o2 + i * 2048, 2048, BF16).rearrange("p (f t) -> p f t", f=2) for i in range(2)]
              o2 += 4096
              wgb = [carve(X, o2 + i * 4096, 4096, BF16).rearrange("p (c f) -> p c f", c=8) for i in range(2)]
              o2 += 8192
              assert o2 <= 22528 * 4
              wub = [carve(mixT_t, i * 4096, 4096, BF16).rearrange("p (c f) -> p c f", c=8) for i in range(2)]
              wdb = [carve(mixT_t, 8192 + i * 4096, 4096, BF16).rearrange("p (c d) -> p c d", c=2) for i in range(2)]
              selst = carve(mixT_t, 16384, 8192, BF16, parts=32).rearrange("p (e m) -> p e m", e=32)
              wstage = carve(mixT_t, 24576, 8192, F32)

              w = load_w(wr_d[:, :, :], 36)
              T.copy(wr[:, :, :], w[:, :, 0:36], eng="pool")
              for half in range(2):
                  T.dma(wstage[0:32, :], sele_d[:, half * 2048:(half + 1) * 2048])
                  T.copy(selst[:, half * 16:(half + 1) * 16, :],
                         wstage[0:32, :].rearrange("p (e m) -> p e m", e=16), eng="pool")
              for t in range(NT):
                  tl = slice(t * 128, (t + 1) * 128)
                  psL = P[t % 2]
                  for c in range(8):
                      T.mm(psL[:, 0:36], h2T[:, c, tl], wr[:, c, :], start=(c == 0), stop=(c == 7))
                  T.copy(lg, psL[:, 0:36])
                  gmax, ngm, sume, pg = sc(1), sc(1), sc(1), sc(1)
                  T.red(gmax, lg[:, 0:4], ALU.max)
                  T.ts(ngm, gmax, -1.0, ALU.mult)
                  eg = sc(4)
                  T.act(eg, lg[:, 0:4], AF.Exp, bias=ngm, scale=1.0, accum_out=sume)
                  T.recip(pg, sume)
                  gsel = sc(4)
                  T.ts(gsel, lg[:, 0:4], gmax, ALU.is_ge, -1.0, ALU.add)
                  T.ts(gsel, gsel, 1e30, ALU.mult)
                  for g in range(4):
                      T.ts(lm[:, g * 8:(g + 1) * 8], lg[:, 4 + g * 8:12 + g * 8], gsel[:, g:g + 1], ALU.add)
                  T.max8(t8, lm)
                  nt1 = sc(1)
                  T.ts(nt1, t8[:, 0:1], -1.0, ALU.mult)
                  T.act(ee, lm, AF.Exp, bias=nt1, scale=1.0)
                  T.ts(selm, lm, t8[:, 1:2], ALU.is_ge)
                  T.tt(ee, ee, selm, ALU.mult)
                  den, scl2 = sc(1), sc(1)
                  T.red(den, ee, ALU.add)
                  T.recip(den, den)
                  T.tt(scl2, den, pg, ALU.mult)
                  T.ts(comb, ee, scl2, ALU.mult)
                  psT = P[2 + t % 2]
                  T.tr(psT[0:32, 0:128], comb, CM["IDN"])
                  T.copy(combT[:, tl], psT[0:32, 0:128], eng="act")

              ck(7, [combT[:, :], lg, lm, ee, selm, comb, t8, small[:, :]])
              ybank = [0]
              pend = None

              def down(e_i, tc, hd, wd):
                  for i in range(4):
                      t = tc * 4 + i
                      for half in range(2):
                          ps = P[5 + ybank[0] % 3]
                          ybank[0] += 1
                          for fc in range(2):
                              T.mm(ps[:, :], hd[:, fc, i * 128:(i + 1) * 128], wd[:, fc, half * 512:(half + 1) * 512],
                                   start=(fc == 0), stop=(fc == 1))
                          ysl = yacc[:, t, half * 512:(half + 1) * 512]
                          T.tt(ysl, ysl, ps[:, :], ALU.add)

              it = 0
              for e_i in range(32):
                  bi = e_i % 2
                  T.dma(wstage[:, :], wg_d[e_i, :, :])
                  T.copy(wgb[bi][:, :, :], wstage[:, :].rearrange("p (c f) -> p c f", c=8), eng="pool")
                  T.dma(wstage[:, :], wu_d[e_i, :, :])
                  T.copy(wub[bi][:, :, :], wstage[:, :].rearrange("p (c f) -> p c f", c=8), eng="pool")
                  T.dma(wstage[:, :], wd_d[e_i, :, :])
                  T.copy(wdb[bi][:, :, :], wstage[:, :].rearrange("p (c d) -> p c d", c=2), eng="pool")
                  for tc in range(4):
                      cs = slice(tc * 512, (tc + 1) * 512)
                      for fc in range(2):
                          for c in range(8):
                              T.mm(P[fc][:, :], wgb[bi][:, c, fc * 128:(fc + 1) * 128], h2T[:, c, cs], start=(c == 0), stop=(c == 7))
                          for c in range(8):
                              T.mm(P[2 + fc][:, :], wub[bi][:, c, fc * 128:(fc + 1) * 128], h2T[:, c, cs], start=(c == 0), stop=(c == 7))
                      T.mm(P[4][:, :], selst[:, e_i, :], combT[:, cs])
                      if pend is not None:
                          down(*pend)
                      hd = hid[it % 2]
                      it += 1
                      for fc in range(2):
                          T.act(sg[:, fc, :], P[fc][:, :], AF.Silu)
                          T.tt(sg[:, fc, :], sg[:, fc, :], P[2 + fc][:, :], ALU.mult)
                          T.tt(hd[:, fc, :], sg[:, fc, :], P[4][:, :], ALU.mult)
                      pend = (e_i, tc, hd, wdb[bi])
              down(*pend)
              pend = None

              ck(8, [yacc[:, 0, :], yacc[:, 9, :]])
              fg = carve(mixT_t, 0, 4096, F32)
              T.dma(fg, fg_d[:, :])
              for t in range(NT):
                  ob = xt[t % 2]
                  rs = rms_rstd(yacc[:, t, :], D, xn[:, :])
                  T.stt(ob[:, :], yacc[:, t, :], rs, fg, ALU.mult, ALU.mult)
                  T.dma(out_d[s, t * 128:(t + 1) * 128, :], ob[:, :])

        except _Done:
            pass
        T.finish()
        T.emit()
    return nc


_NC = None


def _prep_shared(inp):
    f = np.float32
    w_in = np.ascontiguousarray(inp["w_in"][0].reshape(8, 128, 3592).transpose(1, 0, 2), dtype=f)
    w_out = np.ascontiguousarray(inp["w_out"][0].reshape(8, 128, 1024).transpose(1, 0, 2), dtype=f)
    wr = np.concatenate([inp["w_group"][0], inp["w_router"][0]], axis=1)
    w_r = np.ascontiguousarray(wr.reshape(8, 128, 36).transpose(1, 0, 2), dtype=f)
    wg = np.ascontiguousarray(inp["w_gate"][0].reshape(32, 8, 128, 256).transpose(0, 2, 1, 3).reshape(32, 128, 2048), dtype=f)
    wu = np.ascontiguousarray(inp["w_up"][0].reshape(32, 8, 128, 256).transpose(0, 2, 1, 3).reshape(32, 128, 2048), dtype=f)
    wd = np.ascontiguousarray(inp["w_down"][0].reshape(32, 2, 128, 1024).transpose(0, 2, 1, 3).reshape(32, 128, 2048), dtype=f)
    gains = np.concatenate([inp["attn_norm"][0].reshape(8, 128).T, inp["ffn_norm"][0].reshape(8, 128).T], axis=1)
    fg = np.broadcast_to(inp["final_norm"][None, :], (128, D))
    gn4 = np.broadcast_to(np.tile(inp["gdn_norm"][0], 4)[None, :], (128, 512))
    cwv = inp["conv_w"][0].reshape(4, 12, 128).transpose(2, 1, 0).reshape(128, 48)
    dtb = np.broadcast_to(np.tile(inp["dt_bias"][0], 16)[None, :], (128, 64))
    alog = np.broadcast_to(np.tile(inp["A_log"][0], 16)[None, :], (128, 64))
    kind = (np.arange(S)[None, :] // 256 == np.arange(8)[:, None])
    sele = np.zeros((32, 32, 128), f)
    for e in range(32):
        sele[e, e, :] = 1.0
    c = lambda a: np.ascontiguousarray(a, dtype=f)
    return {"w_in": w_in, "w_out": w_out, "w_r": w_r, "w_gate": wg, "w_up": wu, "w_down": wd,
            "cm": _const_mats(), "ac": _attn_consts(), "kind": c(kind), "gains": c(gains), "fgain": c(fg),
            "gn4": c(gn4), "cw": c(cwv), "dtb": c(dtb), "alog": c(alog), "sele": c(sele.reshape(32, 4096))}


def kernel(**inputs):
    global _NC
    inp = {k: np.asarray(v) for k, v in inputs.items()}
    if _NC is None:
        _NC = build_program()
    shared = _prep_shared(inp)
    x = np.ascontiguousarray(inp["x"], dtype=np.float32)
    in_maps = []
    for c in range(8):
        m = dict(shared)
        m["x"] = np.ascontiguousarray(x[c * NSEQ:(c + 1) * NSEQ])
        in_maps.append(m)
    res = run_bass_kernel_spmd(_NC, in_maps, core_ids=list(range(8)))
    out = np.concatenate([np.asarray(r["out"]) for r in res.results], axis=0)
    return out.astype(np.float32, copy=False)
```

```python
import numpy as np
import concourse.bass as bass
import concourse.mybir as mybir
from concourse.bass_utils import run_bass_kernel_spmd

F32 = mybir.dt.float32
BF16 = mybir.dt.bfloat16
AF = mybir.ActivationFunctionType
ALU = mybir.AluOpType
AX = mybir.AxisListType

D = 1024
S = 2048
NT = S // 128
NSEQ = 2
EPS = 1e-6
BIG = 30000.0
_ESZ = {F32: 4, BF16: 2}


def _esz(dt):
    return _ESZ.get(dt, 4)


class Trk:
    def __init__(self, nc, sems, dsems):
        self.nc = nc
        self.sem = sems
        self.dsem = dsems
        self.cnt = {e: 0 for e in ("pe", "act", "dve", "pool")}
        self.duse = [0] * len(dsems)
        self.dnext = 0
        self.ops = {e: [] for e in ("pe", "act", "dve", "pool", "sp")}
        self.waited = {e: {} for e in self.ops}
        self.acc = {}
        self.dma_tokens = []

    @staticmethod
    def box(ap):
        t = ap.tensor
        dims = ap.ap
        es = _esz(ap.dtype)
        if "DRAM" in str(ap.space):
            ext = 1
            for st, c in dims:
                ext += (c - 1) * abs(st)
            return (t.name, 0, 1, ap.offset * es, (ap.offset + ext) * es)
        p0 = ap.start_partition()
        p1 = p0 + ap.partition_size()
        row = dims[0][0]
        f0 = ap.offset - p0 * row if row > 0 else ap.offset
        ext = 1
        for st, c in dims[1:]:
            ext += (c - 1) * abs(st)
        if "PSUM" in str(ap.space):
            return (t.name, (p0 // 32) * 32, ((p1 + 31) // 32) * 32, 0, 2048)
        return (t.name, p0, p1, f0 * es, (f0 + ext) * es)

    @staticmethod
    def _ov(a, b):
        return a[1] < b[2] and b[1] < a[2] and a[3] < b[4] and b[3] < a[4]

    @staticmethod
    def _contains(a, b):
        return a[1] <= b[1] and b[2] <= a[2] and a[3] <= b[3] and b[4] <= a[4]

    def _track(self, engine, outs, ins, token):
        deps = set()
        outs = list(outs) + [a for a in ins if "PSUM" in str(a.space)]
        ins = [a for a in ins if "PSUM" not in str(a.space)]
        rb = [self.box(a) for a in ins]
        wb = [self.box(a) for a in outs]
        for b in rb:
            for r in self.acc.get(b[0], ()):
                if r[2] and self._ov(r[0], b):
                    deps.add(r[1])
        for b in wb:
            for r in self.acc.get(b[0], ()):
                if self._ov(r[0], b):
                    deps.add(r[1])
        for b in wb:
            lst = self.acc.setdefault(b[0], [])
            lst[:] = [r for r in lst if not self._contains(b, r[0])]
            lst.append((b, token, True, engine))
        for b in rb:
            lst = self.acc.setdefault(b[0], [])
            rep = False
            if engine != "dma":
                for i, r in enumerate(lst):
                    if (not r[2]) and r[3] == engine and r[0] == b:
                        lst[i] = (b, token, False, engine)
                        rep = True
                        break
            if not rep:
                lst.append((b, token, False, engine))
        deps.discard(token)
        return deps

    def _waits(self, engine, deps):
        out = []
        w = self.waited[engine]
        best = {}
        for key, val in deps:
            if best.get(key, 0) < val:
                best[key] = val
        for key, val in sorted(best.items(), key=lambda d: str(d[0])):
            if key == "pe" and engine == "pe":
                continue
            if w.get(key, 0) >= val:
                continue
            w[key] = val
            sem = self.sem[key] if isinstance(key, str) else self.dsem[key[1]]
            out.append((sem, val))
        return out

    def op(self, engine, fn, outs, ins, signal=True):
        if signal:
            self.cnt[engine] += 1
            token = (engine, self.cnt[engine])
        else:
            token = (engine, self.cnt[engine] + 1)
        deps = self._track(engine, outs, ins, token)
        self.ops[engine].append((self._waits(engine, deps), fn, (self.sem[engine], 1) if signal else None))

    def dma(self, out, in_, queue="sp", **kw):
        k = self.dnext
        self.dnext = (self.dnext + 1) % len(self.dsem)
        deps = set()
        if self.duse[k] > 0:
            deps.add((("d", k), 16 * self.duse[k]))
        self.duse[k] += 1
        token = (("d", k), 16 * self.duse[k])
        deps |= self._track("dma", [out], [in_], token)
        self.ops[queue].append((self._waits(queue, deps),
                                lambda e, o=out, i=in_, kw=kw: e.dma_start(out=o, in_=i, **kw),
                                (self.dsem[k], 16)))

    def finish(self):
        deps = set()
        for k, u in enumerate(self.duse):
            if u > 0:
                deps.add((("d", k), 16 * u))
        for e in ("pe", "act", "dve", "pool"):
            if self.cnt[e] > 0:
                deps.add((e, self.cnt[e]))
        w = self._waits("sp", deps)
        self.ops["sp"].append((w, None, None))

    def emit(self):
        with self.nc.Block() as block:
            for name, deco in (("sp", block.sync), ("act", block.scalar), ("dve", block.vector),
                               ("pool", block.gpsimd), ("pe", block.tensor)):
                ops = self.ops[name]

                def body(e, ops=ops):
                    for waits, fn, inc in ops:
                        for sem, val in waits:
                            e.wait_ge(sem, val)
                        if fn is None:
                            continue
                        ins = fn(e)
                        if inc is not None:
                            ins.then_inc(inc[0], inc[1])
                deco(body)

    def mm(self, out, lhsT, rhs, start=True, stop=True):
        self.op("pe", lambda e: e.matmul(out, lhsT, rhs, start=start, stop=stop), [out], [lhsT, rhs], signal=stop)

    def tr(self, out, in_, ident):
        self.op("pe", lambda e: e.transpose(out, in_, ident), [out], [in_, ident])

    def act(self, out, in_, func, bias=None, scale=None, accum_out=None):
        kw = {}
        ins = [in_]
        outs = [out]
        if bias is not None:
            kw["bias"] = bias
            if not isinstance(bias, (int, float)):
                ins.append(bias)
        if scale is not None:
            kw["scale"] = scale
            if not isinstance(scale, (int, float)):
                ins.append(scale)
        if accum_out is not None:
            kw["accum_out"] = accum_out
            outs.append(accum_out)
        self.op("act", lambda e: e.activation(out=out, in_=in_, func=func, **kw), outs, ins)

    def ts(self, out, in0, s1, op0, s2=None, op1=None, eng="dve"):
        ins = [in0] + [s for s in (s1, s2) if s is not None and not isinstance(s, (int, float))]
        kw = {}
        if op1 is not None:
            kw["op1"] = op1
        self.op(eng, lambda e: e.tensor_scalar(out=out, in0=in0, scalar1=s1, scalar2=s2, op0=op0, **kw), [out], ins)

    def tt(self, out, in0, in1, op, eng="dve"):
        self.op(eng, lambda e: e.tensor_tensor(out=out, in0=in0, in1=in1, op=op), [out], [in0, in1])

    def stt(self, out, in0, scalar, in1, op0, op1):
        ins = [in0, in1] + ([] if isinstance(scalar, (int, float)) else [scalar])
        self.op("dve", lambda e: e.scalar_tensor_tensor(out=out, in0=in0, scalar=scalar, in1=in1, op0=op0, op1=op1),
                [out], ins)

    def copy(self, out, in_, eng="dve"):
        if eng == "act":
            self.act(out, in_, AF.Copy)
        else:
            self.op(eng, lambda e: e.tensor_copy(out=out, in_=in_), [out], [in_])

    def recip(self, out, in_):
        self.op("dve", lambda e: e.reciprocal(out=out, in_=in_), [out], [in_])

    def red(self, out, in_, op, axis=None):
        ax = AX.X if axis is None else axis
        self.op("dve", lambda e: e.tensor_reduce(out=out, in_=in_, axis=ax, op=op), [out], [in_])

    def max8(self, out, in_):
        self.op("dve", lambda e: e.max(out=out, in_=in_), [out], [in_])

    def memset(self, ap, val, eng="dve"):
        self.op(eng, lambda e: e.memset(ap, val), [ap], [])


CM_NAMES = ["IDN", "ONES", "LTU", "MUS", "MLS", "CA", "CB", "SC", "TRI"]


def _const_mats():
    i = np.arange(128)
    same = (i[:, None] // 64) == (i[None, :] // 64)
    m = {}
    m["IDN"] = np.eye(128)
    m["ONES"] = np.ones((128, 128))
    m["LTU"] = (same & (i[:, None] <= i[None, :]))
    m["MUS"] = (same & (i[:, None] < i[None, :]))
    m["MLS"] = (same & (i[:, None] > i[None, :]))
    m["CA"] = np.broadcast_to((i[:, None] < 64), (128, 128))
    m["CB"] = np.broadcast_to((i[:, None] >= 64), (128, 128))
    m["SC"] = same
    m["TRI"] = (i[:, None] <= i[None, :])
    return np.concatenate([np.asarray(m[n], dtype=np.float32) for n in CM_NAMES], axis=1)


def _attn_consts():
    neg = np.zeros((4, 8, 8), np.float32)
    cst = np.zeros((8, 8, 8), np.float32)
    base = np.zeros((8, 8, 8), np.float32)
    j = np.arange(8)
    for own in range(8):
        cst[own] = np.where(j == own, 0.0, -BIG)[None, :]
        base[own] = np.where(j <= own, 0.0, -BIG)[None, :]
        if own >= 4:
            neg[own - 4] = np.where(j < own, 0.0, -1e30)[None, :]
    row = np.concatenate([neg.reshape(-1), cst.reshape(-1), base.reshape(-1)])
    return np.broadcast_to(row[None, :], (128, row.size)).astype(np.float32).copy()


class _Done(Exception):
    pass


def build_program(stage=0, nseq=NSEQ):
    nc = bass.Bass("TRN2", target_bir_lowering=False)
    dbg_d = nc.dram_tensor("dbg", [16, 128, 2048], F32, kind="ExternalOutput").ap() if stage else None

    def din(name, shape):
        return nc.dram_tensor(name, list(shape), F32, kind="ExternalInput").ap()

    x_d = din("x", [NSEQ, S, D])
    win_d = din("w_in", [128, 8, 3592])
    wout_d = din("w_out", [128, 8, 1024])
    wr_d = din("w_r", [128, 8, 36])
    ne_decl = 32 if stage in (0, 6, 7, 8) else 1
    wg_d = din("w_gate", [ne_decl, 128, 8 * 256])
    wu_d = din("w_up", [ne_decl, 128, 8 * 256])
    wd_d = din("w_down", [ne_decl, 128, 2 * 1024])
    cm_d = din("cm", [128, 9 * 128])
    ac_d = din("ac", [128, 256 + 512 + 512])
    kind_d = din("kind", [8, S])
    gains_d = din("gains", [128, 16])
    fg_d = din("fgain", [128, D])
    gn_d = din("gn4", [128, 512])
    cw_d = din("cw", [128, 48])
    dtb_d = din("dtb", [128, 64])
    alog_d = din("alog", [128, 64])
    sele_d = din("sele", [32, 32 * 128])
    out_d = nc.dram_tensor("out", [NSEQ, S, D], F32, kind="ExternalOutput").ap()

    import contextlib
    es = contextlib.ExitStack()
    with es:
        def sb(name, shape, dt=F32):
            return es.enter_context(nc.sbuf_tensor("sb_" + name, list(shape), dt))

        def sem(name):
            return es.enter_context(nc.semaphore(name))

        sems = {e: sem("s_" + e) for e in ("pe", "act", "dve", "pool")}
        dsems = [sem("d%d" % i) for i in range(24)]
        T = Trk(nc, sems, dsems)

        cm = sb("cm", [128, 9 * 128])
        CM = {n: cm[:, i * 128:(i + 1) * 128] for i, n in enumerate(CM_NAMES)}
        ac = sb("ac", [128, 1280])
        negmask = ac[:, 0:256].rearrange("p (o c) -> p o c", o=4)
        cstb = ac[:, 256:768].rearrange("p (o c) -> p o c", o=8)
        basebias = ac[:, 768:1280].rearrange("p (o c) -> p o c", o=8)
        idb = sb("idb", [128, 128], BF16)
        trib = sb("trib", [128, 128], BF16)
        idn4 = sb("idn4", [128, 4, 128])
        ltu4 = sb("ltu4", [128, 4, 128])
        gains = sb("gains", [128, 16])
        gn4 = sb("gn4", [128, 512])
        cw = sb("cw", [128, 48])
        dtb = sb("dtb", [128, 64])
        nega = sb("nega", [128, 64])
        gsm = sb("gsm", [128, 128])
        small = sb("small", [128, 256])
        xt = [sb("xt%d" % i, [128, D]) for i in range(2)]
        xn = sb("xn", [128, D], BF16)
        wst = [sb("wst%d" % i, [128, 8, 128]) for i in range(2)]
        wbf = [sb("wbf%d" % i, [128, 8, 128], BF16) for i in range(2)]
        hT = sb("hT", [128, 8 * S], BF16)
        mixT_t = sb("mixT", [128, 8 * S], BF16)
        X = sb("X", [128, 22528])
        P = [es.enter_context(nc.psum_tensor("ps%d" % i, [128, 512], F32)) for i in range(8)]

        hT3 = hT[:, :].rearrange("p (c t) -> p c t", c=8)
        mixT = mixT_t[:, :].rearrange("p (c t) -> p c t", c=8)

        def carve(arena, off_b, nbytes, dt, parts=128):
            aes = _esz(arena.dtype)
            v = arena[0:parts, off_b // aes:(off_b + nbytes) // aes]
            if dt != arena.dtype:
                v = v.bitcast(dt)
            return v

        def pbf(bank):
            return P[bank][:, :].bitcast(BF16)

        sc_i = [0]

        def sc(n):
            if sc_i[0] + n > 256:
                sc_i[0] = 0
            a = small[:, sc_i[0]:sc_i[0] + n]
            sc_i[0] += n
            return a

        T.dma(cm[:, :], cm_d[:, :])
        T.dma(ac[:, :], ac_d[:, :])
        T.dma(gains[:, :], gains_d[:, :])
        T.dma(gn4[:, :], gn_d[:, :])
        T.dma(cw[:, :], cw_d[:, :])
        T.dma(dtb[:, :], dtb_d[:, :])
        T.dma(nega[:, :], alog_d[:, :])
        T.copy(idb[:, :], CM["IDN"])
        T.copy(trib[:, :], CM["TRI"])
        for h in range(4):
            T.copy(idn4[:, h, :], CM["IDN"])
            T.copy(ltu4[:, h, :], CM["LTU"])
        T.act(nega[:, :], nega[:, :], AF.Exp)
        T.ts(nega[:, :], nega[:, :], -1.0, ALU.mult)

        wload_i = [0]

        def load_w(src_ap, ncols):
            i = wload_i[0] % 2
            wload_i[0] += 1
            T.dma(wst[i][:, :, 0:ncols], src_ap)
            T.copy(wbf[i][:, :, 0:ncols], wst[i][:, :, 0:ncols], eng="pool")
            return wbf[i]

        def rms_rstd(src, n, junk):
            ss = sc(1)
            T.act(junk, src, AF.Square, accum_out=ss)
            sd = sc(1)
            T.act(sd, ss, AF.Sqrt, bias=epsc[:, 0:1], scale=1.0 / n)
            rs = sc(1)
            T.recip(rs, sd)
            return rs

        epsc = sb("epsc", [128, 2])
        T.memset(epsc[:, 0:1], EPS)
        T.memset(epsc[:, 1:2], 1.0)

        def norm_T(src, gcol0, dst3, tok0):
            rs = rms_rstd(src, D, xn[:, :])
            T.ts(xn[:, :], src, rs, ALU.mult)
            pt = pbf(7).rearrange("p (c t) -> p c t", c=8)
            for c in range(8):
                T.tr(pt[:, c, :], xn[:, c * 128:(c + 1) * 128], idb[:, :])
            for c in range(8):
                T.act(dst3[:, c, tok0:tok0 + 128], pt[:, c, :], AF.Copy, scale=gains[:, gcol0 + c:gcol0 + c + 1])

        dbgn = [0]
        dbgst = sb("dbgst", [128, 2048]) if stage else None

        def ck(k, aps):
            if stage != k:
                return
            for a in aps:
                n = a.shape[-1]
                p = a.shape[0]
                T.copy(dbgst[0:p, 0:n], a)
                T.dma(dbg_d[dbgn[0], 0:p, 0:n], dbgst[0:p, 0:n])
                dbgn[0] += 1
            raise _Done()

        try:
          for s in range(nseq):
              for t in range(NT):
                  xb = xt[t % 2]
                  T.dma(xb[:, :], x_d[s, t * 128:(t + 1) * 128, :])
                  norm_T(xb[:, :], 0, hT3, t * 128)

              ck(1, [hT3[:, 0, :], hT3[:, 7, :]])
              qa = carve(X, 0, 32768, BF16, parts=72).rearrange("p (h t) -> p h t", h=8)
              ka = carve(X, 32768, 32768, BF16, parts=72).rearrange("p (h t) -> p h t", h=8)
              vaug = carve(X, 65536, 16 * 8 * 65 * 2, BF16).rearrange("p (t h e) -> p t h e", t=16, h=8)
              o2 = 65536 + 16 * 8 * 65 * 2
              ya = [carve(X, o2 + i * 1024, 1024, BF16) for i in range(4)]
              o2 += 4096
              pTb = [carve(X, o2 + i * 1024, 1024, BF16) for i in range(2)]
              o2 += 2048
              kms = carve(X, o2, 256, F32, parts=64).rearrange("p (h j) -> p h j", h=8)
              o2 += 256
              kmb = carve(X, o2, 128, BF16, parts=64).rearrange("p (h j) -> p h j", h=8)
              o2 += 128
              gm = carve(X, o2, 256, F32)
              o2 += 256
              top = carve(X, o2, 256, F32).rearrange("p (h j) -> p h j", h=8)
              o2 += 256
              selb = carve(X, o2, 256, F32)
              o2 += 256
              sbias = carve(X, o2, 128, BF16)
              o2 += 128
              stg = carve(X, o2, 256, BF16, parts=64)
              o2 += 256

              T.memset(vaug[:, :, :, 64:65], 1.0)
              for half in range(2):
                  T.dma(xt[0][64:72, :], kind_d[:, half * 1024:(half + 1) * 1024])
                  for h in range(8):
                      T.copy(ka[64:72, h, half * 1024:(half + 1) * 1024], xt[0][64:72, :], eng="pool")

              for which, dst in ((0, qa), (1, ka)):
                  for g2 in range(4):
                      c0 = which * 512 + g2 * 128
                      w = load_w(win_d[:, :, c0:c0 + 128], 128)
                      for hh in range(2):
                          h = g2 * 2 + hh
                          for tc in range(4):
                              ps = P[(h * 4 + tc) % 2]
                              for c in range(8):
                                  T.mm(ps[0:64, :], w[:, c, hh * 64:(hh + 1) * 64], hT3[:, c, tc * 512:(tc + 1) * 512],
                                       start=(c == 0), stop=(c == 7))
                              T.copy(dst[0:64, h, tc * 512:(tc + 1) * 512], ps[0:64, :], eng=("act" if tc % 2 else "dve"))
              for g2 in range(4):
                  c0 = 1024 + g2 * 128
                  w = load_w(win_d[:, :, c0:c0 + 128], 128)
                  for t in range(NT):
                      ps = P[t % 2]
                      for c in range(8):
                          T.mm(ps[:, 0:128], hT3[:, c, t * 128:(t + 1) * 128], w[:, c, :], start=(c == 0), stop=(c == 7))
                      T.copy(vaug[:, t, g2 * 2:g2 * 2 + 2, 0:64], ps[:, 0:128].rearrange("p (h e) -> p h e", h=2),
                             eng=("act" if t % 2 else "dve"))
              for h in range(8):
                  T.red(kms[:, h, :], ka[0:64, h, :].rearrange("p (j l) -> p j l", j=8), ALU.add)
              T.ts(kmb[:, :, :], kms[:, :, :], 1.0 / 256.0, ALU.mult)
              for qt in range(NT):
                  own = qt // 2
                  if own >= 4:
                      psg = P[6]
                      for h in range(8):
                          T.mm(psg[:, h * 8:(h + 1) * 8], qa[0:64, h, qt * 128:(qt + 1) * 128], kmb[:, h, :])
                      T.tt(gm, psg[:, 0:64], negmask[:, own - 4, :], ALU.add)
                      gm3 = gm.rearrange("p (h j) -> p h j", h=8)
                      for h in range(8):
                          T.max8(top[:, h, :], gm3[:, h, :])
                      for h in range(8):
                          T.ts(selb[:, h * 8:(h + 1) * 8], gm3[:, h, :], top[:, h, 2:3], ALU.is_ge)
                      T.stt(sbias, selb, BIG, cstb[:, own, :], ALU.mult, ALU.add)
                  else:
                      T.copy(sbias, basebias[:, own, :])
                  pt = pbf(7)
                  T.tr(pt[0:64, 0:128], sbias, idb[:, :])
                  T.copy(stg, pt[0:64, 0:128])
                  for h in range(8):
                      T.dma(qa[64:72, h, qt * 128:(qt + 1) * 128], stg[h * 8:(h + 1) * 8, :])
              ck(2, [qa[:, 0, :], qa[:, 5, :], ka[:, 0, :], ka[:, 5, :], vaug[:, 3, :, :].rearrange('p h e -> p (h e)'), kmb[:, :, :].rearrange('p h j -> p (h j)')])
              for qc in range(4):
                  for h in range(8):
                      nkt = 4 * qc + 4

                      def score(kt, qc=qc, h=h):
                          i0 = max(0, kt - 4 * qc)
                          ncol = (4 - i0) * 128
                          c0 = qc * 512 + i0 * 128
                          ps = P[kt % 2]
                          T.mm(ps[:, 0:ncol], ka[0:72, h, kt * 128:(kt + 1) * 128], qa[0:72, h, c0:c0 + ncol])
                          pT = pTb[kt % 2]
                          T.act(pT[:, 0:ncol], ps[:, 0:ncol], AF.Exp, scale=0.125)
                          if kt >= 4 * qc:
                              T.tt(pT[:, 0:128], pT[:, 0:128], trib[:, :], ALU.mult, eng="pool")

                      score(0)
                      for kt in range(nkt):
                          if kt + 1 < nkt:
                              score(kt + 1)
                          i0 = max(0, kt - 4 * qc)
                          pT = pTb[kt % 2]
                          for i in range(i0, 4):
                              qt = 4 * qc + i
                              T.mm(P[2 + i][:, 0:65], pT[:, (i - i0) * 128:(i - i0 + 1) * 128], vaug[:, kt, h, :],
                                   start=(kt == 0), stop=(kt == qt))
                      for i in range(4):
                          rec = sc(1)
                          T.recip(rec, P[2 + i][:, 64:65])
                          T.act(ya[i][:, h * 64:(h + 1) * 64], P[2 + i][:, 0:64], AF.Copy, scale=rec)
                  for i in range(4):
                      qt = 4 * qc + i
                      pt = pbf(7).rearrange("p (c t) -> p c t", c=8)
                      for c in range(4):
                          T.tr(pt[:, c, :], ya[i][:, c * 128:(c + 1) * 128], idb[:, :])
                      T.copy(mixT[:, 0:4, qt * 128:(qt + 1) * 128], pt[:, 0:4, :])

              ck(3, [mixT[:, 0, :], mixT[:, 3, :]])
              qnT = carve(X, 0, 16384, BF16).rearrange("p (h t) -> p h t", h=4)
              knT = carve(X, 16384, 16384, BF16).rearrange("p (h t) -> p h t", h=4)
              vT = carve(X, 32768, 16384, BF16).rearrange("p (h t) -> p h t", h=4)
              zs = carve(X, 49152, 16384, BF16).rearrange("p (t e) -> p t e", t=16)
              o2 = 65536
              pre = carve(X, o2, 2052 * 4, F32)
              o2 += 2052 * 4
              cacc = carve(X, o2, 8192, F32)
              o2 += 8192
              tsq = carve(X, o2, 2048, F32)
              o2 += 2048
              tsd = carve(X, o2, 2048, F32)
              o2 += 2048
              gt = carve(X, o2, 256, F32).rearrange("p (t h) -> p t h", t=16)
              o2 += 256
              bet = carve(X, o2, 256, F32).rearrange("p (t h) -> p t h", t=16)
              o2 += 256
              xab = carve(X, o2, 256, F32)
              o2 += 256
              T.memset(pre[:, 0:3], 0.0)

              for ch in range(12):
                  c0 = 1536 + ch * 128
                  w = load_w(win_d[:, :, c0:c0 + 128], 128)
                  for tc in range(4):
                      ps = P[tc % 2]
                      for c in range(8):
                          T.mm(ps[:, :], w[:, c, :], hT3[:, c, tc * 512:(tc + 1) * 512], start=(c == 0), stop=(c == 7))
                      T.copy(pre[:, 3 + tc * 512:3 + (tc + 1) * 512], ps[:, :], eng="act")
                  T.ts(cacc, pre[:, 0:S], cw[:, ch * 4:ch * 4 + 1], ALU.mult)
                  for i in range(1, 4):
                      T.stt(cacc, pre[:, i:i + S], cw[:, ch * 4 + i:ch * 4 + i + 1], cacc, ALU.mult, ALU.add)
                  kind, h = ch // 4, ch % 4
                  if kind == 2:
                      T.act(vT[:, h, :], cacc, AF.Silu)
                  else:
                      T.act(cacc, cacc, AF.Silu)
                      dst = qnT if kind == 0 else knT
                      scl = (128.0 ** -0.5) if kind == 0 else 1.0
                      for tc in range(4):
                          sl = slice(tc * 512, (tc + 1) * 512)
                          T.act(tsq, cacc[:, sl], AF.Square)
                          psn = P[2 + tc % 2]
                          T.mm(psn[:, :], CM["ONES"], tsq)
                          T.act(tsd, psn[:, :], AF.Sqrt, bias=epsc[:, 0:1], scale=1.0)
                          T.recip(tsd, tsd)
                          T.stt(dst[:, h, sl], cacc[:, sl], scl, tsd, ALU.mult, ALU.mult)
              for g2 in range(4):
                  c0 = 3072 + g2 * 128
                  w = load_w(win_d[:, :, c0:c0 + 128], 128)
                  for t in range(NT):
                      ps = P[t % 2]
                      for c in range(8):
                          T.mm(ps[:, 0:128], hT3[:, c, t * 128:(t + 1) * 128], w[:, c, :], start=(c == 0), stop=(c == 7))
                      T.act(zs[:, t, g2 * 128:(g2 + 1) * 128], ps[:, 0:128], AF.Silu)
              w = load_w(win_d[:, :, 3584:3592], 8)
              psab = P[4]
              for t in range(NT):
                  for c in range(8):
                      T.mm(psab[:, t * 8:(t + 1) * 8], hT3[:, c, t * 128:(t + 1) * 128], w[:, c, 0:8],
                           start=(c == 0), stop=(c == 7))
              pab3 = psab[:, 0:128].rearrange("p (t e) -> p t e", t=16)
              xab3 = xab.rearrange("p (t h) -> p t h", t=16)
              T.tt(xab3, pab3[:, :, 0:4], dtb[:, :].rearrange("p (t h) -> p t h", t=16), ALU.add)
              T.act(xab, xab, AF.Exp)
              T.act(xab, xab, AF.Ln, bias=epsc[:, 1:2], scale=1.0)
              T.tt(gt, xab3, nega[:, :].rearrange("p (t h) -> p t h", t=16), ALU.mult)
              T.act(bet, pab3[:, :, 4:8], AF.Sigmoid)

              ck(4, [qnT[:, 0, :], knT[:, 2, :], vT[:, 1, :], zs[:, 5, :], gt[:, :, :].rearrange('p t h -> p (t h)'), bet[:, :, :].rearrange('p t h -> p (t h)')])
              hoff = [0]

              def hc(nbytes, dt, shape4=True):
                  v = carve(hT, hoff[0], nbytes, dt)
                  hoff[0] += nbytes
                  return v.rearrange("p (h e) -> p h e", h=4) if shape4 else v

              gbc = hc(2048, F32)
              dt4 = hc(2048, F32)
              dl4 = hc(2048, F32)
              A4 = hc(2048, F32)
              qkm4 = hc(1024, BF16)
              qb = [hc(1024, BF16) for _ in range(2)]
              qtb = [hc(1024, BF16) for _ in range(2)]
              pb = [hc(1024, BF16) for _ in range(2)]
              bgk = hc(1024, BF16)
              kdec = hc(1024, BF16)
              bv = hc(1024, BF16)
              uv = hc(2048, F32)
              wT = hc(1024, BF16)
              ub = hc(1024, BF16)
              o1s = hc(2048, F32)
              ot = hc(2048, F32)
              S32 = hc(2048, F32)
              Sb = hc(1024, BF16)
              zg = hc(2048, F32)
              yb = hc(1024, BF16)
              Gs = gsm[:, 0:16]
              ex = gsm[:, 16:48]
              assert hoff[0] <= 32768
              T.memset(S32[:, :, :], 0.0)
              T.memset(Sb[:, :, :], 0.0)
              T.memset(ub[:, :, :], 0.0)
              xo = [65536]

              def xc(nbytes, dt):
                  v = carve(X, xo[0], nbytes, dt)
                  xo[0] += nbytes
                  return v.rearrange("p (h e) -> p h e", h=4)

              uv2 = [uv, xc(2048, F32)]
              wT2 = [wT, xc(1024, BF16)]
              kdA2 = [kdec, xc(1024, BF16)]
              kdB2 = [xc(1024, BF16), xc(1024, BF16)]
              qkm2 = [qkm4, xc(1024, BF16)]
              assert xo[0] <= 65536 + 20000

              def b4(bank):
                  return P[bank][:, :].rearrange("p (h e) -> p h e", h=4)

              def pre(t):
                  bi = t % 2
                  tl = slice(t * 128, (t + 1) * 128)
                  g_t, b_t = gt[:, t, :], bet[:, t, :]
                  e0 = 16 + bi * 40
                  Gs_ = gsm[:, e0:e0 + 16]
                  exx = gsm[:, e0 + 16:e0 + 40]
                  gam, glab, kds, bg = exx[:, 0:4], exx[:, 4:12], exx[:, 12:16], exx[:, 16:20]
                  kdsA, kdsB = exx[:, 20:22], exx[:, 22:24]
                  d1 = gsm[:, 0:4]
                  kdsAB = gsm[:, 4:12]
                  psG = P[0]
                  for i, mname in enumerate(("LTU", "SC", "CA", "CB")):
                      T.mm(psG[:, i * 4:(i + 1) * 4], CM[mname], g_t)
                  T.copy(Gs_, psG[:, 0:16])
                  yield
                  T.act(gam, Gs_[:, 0:4], AF.Exp)
                  T.act(glab, Gs_[:, 8:16], AF.Exp)
                  T.tt(d1, Gs_[:, 4:8], Gs_[:, 0:4], ALU.subtract)
                  T.act(kds, d1, AF.Exp)
                  T.tt(bg, b_t, gam, ALU.mult)
                  T.ts(kdsAB[:, 0:4], kds, CM["CA"][:, 0:1], ALU.mult)
                  T.ts(kdsAB[:, 4:8], kds, CM["CB"][:, 0:1], ALU.mult)
                  yield
                  psR = b4(1)
                  for h in range(4):
                      T.ts(gbc[:, h, :], CM["ONES"], g_t[:, h:h + 1], ALU.mult)
                      T.mm(psR[:, h, :], gbc[:, h, :], CM["LTU"])
                  yield
                  for h in range(4):
                      T.ts(dt4[:, h, :], psR[:, h, :], Gs_[:, h:h + 1], ALU.subtract, 0.0, ALU.min)
                      T.ts(dl4[:, h, :], psR[:, h, :], Gs_[:, h:h + 1], ALU.subtract, 0.0, ALU.max)
                  yield
                  T.act(dt4[:, :, :], dt4[:, :, :], AF.Exp)
                  T.act(dl4[:, :, :], dl4[:, :, :], AF.Exp, scale=-1.0)
                  psK, psQ = b4(2), b4(3)
                  for h in range(4):
                      T.mm(psK[:, h, :], knT[:, h, tl], knT[:, h, tl])
                      T.mm(psQ[:, h, :], knT[:, h, tl], qnT[:, h, tl])
                  yield
                  T.tt(A4[:, :, :], psK, dl4[:, :, :], ALU.mult)
                  yield
                  for h in range(4):
                      T.stt(A4[:, h, :], A4[:, h, :], b_t[:, h:h + 1], CM["MLS"], ALU.mult, ALU.mult)
                  yield
                  T.tt(dt4[:, :, :], psQ, dt4[:, :, :], ALU.mult)
                  T.tt(qkm2[bi][:, :, :], dt4[:, :, :], ltu4[:, :, :], ALU.mult)
                  psN = b4(0)
                  for h in range(4):
                      T.tr(psN[:, h, :], A4[:, h, :], CM["IDN"])
                  yield
                  T.copy(qtb[0][:, :, :], A4[:, :, :], eng="pool")
                  T.copy(qb[0][:, :, :], psN, eng="act")
                  T.tt(pb[0][:, :, :], idn4[:, :, :], psN, ALU.subtract)
                  yield
                  cq, cp = 0, 0
                  for k in range(1, 6):
                      psA = b4(1)
                      for h in range(4):
                          T.mm(psA[:, h, :], qb[cq][:, h, :], qtb[cq][:, h, :])
                      if k < 5:
                          psB = b4(2)
                          for h in range(4):
                              T.mm(psB[:, h, :], qtb[cq][:, h, :], qb[cq][:, h, :])
                      yield
                      T.copy(qtb[1 - cq][:, :, :], psA, eng="act")
                      if k < 5:
                          T.copy(qb[1 - cq][:, :, :], psB)
                      yield
                      psC = b4(3)
                      for h in range(4):
                          T.mm(psC[:, h, :], qtb[1 - cq][:, h, :], pb[cp][:, h, :])
                      yield
                      T.tt(pb[1 - cp][:, :, :], pb[cp][:, :, :], psC, ALU.add)
                      yield
                      cq, cp = 1 - cq, 1 - cp
                  TT = pb[cp]
                  ptk = pbf(0)[:, 0:512].rearrange('p (h e) -> p h e', h=4)
                  ptv = pbf(0)[:, 512:1024].rearrange('p (h e) -> p h e', h=4)
                  for h in range(4):
                      T.tr(ptk[:, h, :], knT[:, h, tl], idb[:, :])
                      T.tr(ptv[:, h, :], vT[:, h, tl], idb[:, :])
                  yield
                  for h in range(4):
                      T.act(bgk[:, h, :], ptk[:, h, :], AF.Copy, scale=bg[:, h:h + 1])
                      T.ts(kdA2[bi][:, h, :], ptk[:, h, :], kdsAB[:, h:h + 1], ALU.mult)
                      T.ts(kdB2[bi][:, h, :], ptk[:, h, :], kdsAB[:, 4 + h:5 + h], ALU.mult)
                      T.act(bv[:, h, :], ptv[:, h, :], AF.Copy, scale=b_t[:, h:h + 1])
                  yield
                  psU, psW = b4(1), b4(2)
                  for h in range(4):
                      T.mm(psU[:, h, :], TT[:, h, :], bv[:, h, :])
                      T.mm(psW[:, h, :], bgk[:, h, :], TT[:, h, :])
                  yield
                  T.copy(uv2[bi][:, :, :], psU)
                  T.copy(wT2[bi][:, :, :], psW, eng="act")
                  yield

              def rec(t):
                  bi = t % 2
                  tl = slice(t * 128, (t + 1) * 128)
                  e0 = 16 + bi * 40
                  exx = gsm[:, e0 + 16:e0 + 40]
                  gam, glab = exx[:, 0:4], exx[:, 4:12]
                  ss4, rs4 = gsm[:, 12:16], gsm[:, 96:100]
                  uvb, wTb, qkmb = uv2[bi], wT2[bi], qkm2[bi]
                  for xi in range(2):
                      r = slice(xi * 64, (xi + 1) * 64)
                      psWS, psO1, psO2, psS = b4(4), b4(5), b4(6), b4(7)
                      kd = kdA2[bi] if xi == 0 else kdB2[bi]
                      for h in range(4):
                          T.mm(psWS[:, h, :], wTb[:, h, :], Sb[:, h, :])
                      for h in range(4):
                          T.mm(psO1[:, h, :], qnT[:, h, tl], Sb[:, h, :])
                      yield
                      T.tt(ub[r, :, :], uvb[r, :, :], psWS[r, :, :], ALU.subtract)
                      for h in range(4):
                          T.act(o1s[r, h, :], psO1[r, h, :], AF.Copy, scale=gam[r, h:h + 1])
                      yield
                      for h in range(4):
                          T.mm(psS[:, h, :], kd[:, h, :], ub[:, h, :])
                      for h in range(4):
                          T.mm(psO2[:, h, :], qkmb[:, h, :], ub[:, h, :])
                      yield
                      for h in range(4):
                          T.stt(S32[:, h, :], S32[:, h, :], glab[:, xi * 4 + h:xi * 4 + h + 1], psS[:, h, :],
                                ALU.mult, ALU.add)
                      yield
                      T.copy(Sb[:, :, :], S32[:, :, :], eng="pool")
                      T.tt(ot[r, :, :], o1s[r, :, :], psO2[r, :, :], ALU.add)
                      yield
                  for h in range(4):
                      T.act(o1s[:, h, :], ot[:, h, :], AF.Square, accum_out=ss4[:, h:h + 1])
                  T.act(rs4, ss4, AF.Sqrt, bias=epsc[:, 0:1], scale=1.0 / 128.0)
                  T.recip(rs4, rs4)
                  yield
                  T.tt(zg[:, :, :], zs[:, t, :].rearrange("p (h e) -> p h e", h=4),
                       gn4[:, :].rearrange("p (h e) -> p h e", h=4), ALU.mult)
                  for h in range(4):
                      T.stt(yb[:, h, :], ot[:, h, :], rs4[:, h:h + 1], zg[:, h, :], ALU.mult, ALU.mult)
                  yield
                  pty = pbf(4)[:, 0:512].rearrange('p (h e) -> p h e', h=4)
                  for h in range(4):
                      T.tr(pty[:, h, :], yb[:, h, :], idb[:, :])
                  T.copy(mixT[:, 4:8, tl], pty)
                  yield

              def drive(g1, g2):
                  gens = [g for g in (g1, g2) if g is not None]
                  while gens:
                      for g in list(gens):
                          try:
                              next(g)
                          except StopIteration:
                              gens.remove(g)

              drive(pre(0), None)
              for t in range(NT):
                  drive(rec(t), pre(t + 1) if t + 1 < NT else None)

              ck(5, [mixT[:, 4, :], mixT[:, 7, :]])
              yacc = carve(X, 0, 65536, F32).rearrange("p (t d) -> p t d", t=16)
              wo = carve(X, 65536, 16384, BF16).rearrange("p (c d) -> p c d", c=8)
              for g2 in range(8):
                  w = load_w(wout_d[:, :, g2 * 128:(g2 + 1) * 128], 128)
                  T.copy(wo[:, :, g2 * 128:(g2 + 1) * 128], w[:, :, :], eng="pool")
              for t in range(NT):
                  tl = slice(t * 128, (t + 1) * 128)
                  xb = xt[t % 2]
                  T.dma(xb[:, :], x_d[s, tl, :])
                  for half in range(2):
                      ps = P[half + 2 * (t % 2)]
                      for c in range(8):
                          T.mm(ps[:, :], mixT[:, c, tl], wo[:, c, half * 512:(half + 1) * 512], start=(c == 0), stop=(c == 7))
                      T.tt(yacc[:, t, half * 512:(half + 1) * 512], xb[:, half * 512:(half + 1) * 512], ps[:, :], ALU.add)
                  norm_T(yacc[:, t, :], 8, hT3, t * 128)

              ck(6, [yacc[:, 0, :], yacc[:, 9, :], hT3[:, 2, :]])
              h2T = hT3
              o2 = 65536
              wr = carve(X, o2, 8 * 36 * 2, BF16).rearrange("p (c e) -> p c e", c=8)
              o2 += 8 * 36 * 2
              combT = carve(X, o2, 4096, BF16, parts=32)
              o2 += 4096
              lg = carve(X, o2, 144, F32)
              o2 += 144
              lm = carve(X, o2, 128, F32)
              o2 += 128
              ee = carve(X, o2, 128, F32)
              o2 += 128
              selm = carve(X, o2, 128, F32)
              o2 += 128
              comb = carve(X, o2, 128, F32)
              o2 += 128
              t8 = carve(X, o2, 32, F32)
              o2 += 32
              sg = carve(X, o2, 4096, F32).rearrange("p (f t) -> p f t", f=2)
              o2 += 4096
              hid = [carve(X, o2 + i * 2048, 2048, BF16).rearrange("p (f t) -> p f t", f=2) for i in range(2)]
              o2 += 4096
              wgb = [carve(X, o2 + i * 4096, 4096, BF16).rearrange("p (c f) -> p c f", c=8) for i in range(2)]
              o2 += 8192
              assert o2 <= 22528 * 4
              wub = [carve(mixT_t, i * 4096, 4096, BF16).rearrange("p (c f) -> p c f", c=8) for i in range(2)]
              wdb = [carve(mixT_t, 8192 + i * 4096, 4096, BF16).rearrange("p (c d) -> p c d", c=2) for i in range(2)]
              selst = carve(mixT_t, 16384, 8192, BF16, parts=32).rearrange("p (e m) -> p e m", e=32)
              wstage = carve(mixT_t, 24576, 8192, F32)

              w = load_w(wr_d[:, :, :], 36)
              T.copy(wr[:, :, :], w[:, :, 0:36], eng="pool")
              for half in range(2):
                  T.dma(wstage[0:32, :], sele_d[:, half * 2048:(half + 1) * 2048])
                  T.copy(selst[:, half * 16:(half + 1) * 16, :],
                         wstage[0:32, :].rearrange("p (e m) -> p e m", e=16), eng="pool")
              for t in range(NT):
                  tl = slice(t * 128, (t + 1) * 128)
                  psL = P[t % 2]
                  for c in range(8):
                      T.mm(psL[:, 0:36], h2T[:, c, tl], wr[:, c, :], start=(c == 0), stop=(c == 7))
                  T.copy(lg, psL[:, 0:36])
                  gmax, ngm, sume, pg = sc(1), sc(1), sc(1), sc(1)
                  T.red(gmax, lg[:, 0:4], ALU.max)
                  T.ts(ngm, gmax, -1.0, ALU.mult)
                  eg = sc(4)
                  T.act(eg, lg[:, 0:4], AF.Exp, bias=ngm, scale=1.0, accum_out=sume)
                  T.recip(pg, sume)
                  gsel = sc(4)
                  T.ts(gsel, lg[:, 0:4], gmax, ALU.is_ge, -1.0, ALU.add)
                  T.ts(gsel, gsel, 1e30, ALU.mult)
                  for g in range(4):
                      T.ts(lm[:, g * 8:(g + 1) * 8], lg[:, 4 + g * 8:12 + g * 8], gsel[:, g:g + 1], ALU.add)
                  T.max8(t8, lm)
                  nt1 = sc(1)
                  T.ts(nt1, t8[:, 0:1], -1.0, ALU.mult)
                  T.act(ee, lm, AF.Exp, bias=nt1, scale=1.0)
                  T.ts(selm, lm, t8[:, 1:2], ALU.is_ge)
                  T.tt(ee, ee, selm, ALU.mult)
                  den, scl2 = sc(1), sc(1)
                  T.red(den, ee, ALU.add)
                  T.recip(den, den)
                  T.tt(scl2, den, pg, ALU.mult)
                  T.ts(comb, ee, scl2, ALU.mult)
                  psT = P[2 + t % 2]
                  T.tr(psT[0:32, 0:128], comb, CM["IDN"])
                  T.copy(combT[:, tl], psT[0:32, 0:128], eng="act")

              ck(7, [combT[:, :], lg, lm, ee, selm, comb, t8, small[:, :]])
              ybank = [0]
              pend = None

              def down(e_i, tc, hd, wd):
                  for i in range(4):
                      t = tc * 4 + i
                      for half in range(2):
                          ps = P[5 + ybank[0] % 3]
                          ybank[0] += 1
                          for fc in range(2):
                              T.mm(ps[:, :], hd[:, fc, i * 128:(i + 1) * 128], wd[:, fc, half * 512:(half + 1) * 512],
                                   start=(fc == 0), stop=(fc == 1))
                          ysl = yacc[:, t, half * 512:(half + 1) * 512]
                          T.tt(ysl, ysl, ps[:, :], ALU.add)

              it = 0
              for e_i in range(32):
                  bi = e_i % 2
                  T.dma(wstage[:, :], wg_d[e_i, :, :])
                  T.copy(wgb[bi][:, :, :], wstage[:, :].rearrange("p (c f) -> p c f", c=8), eng="pool")
                  T.dma(wstage[:, :], wu_d[e_i, :, :])
                  T.copy(wub[bi][:, :, :], wstage[:, :].rearrange("p (c f) -> p c f", c=8), eng="pool")
                  T.dma(wstage[:, :], wd_d[e_i, :, :])
                  T.copy(wdb[bi][:, :, :], wstage[:, :].rearrange("p (c d) -> p c d", c=2), eng="pool")
                  for tc in range(4):
                      cs = slice(tc * 512, (tc + 1) * 512)
                      for fc in range(2):
                          for c in range(8):
                              T.mm(P[fc][:, :], wgb[bi][:, c, fc * 128:(fc + 1) * 128], h2T[:, c, cs], start=(c == 0), stop=(c == 7))
                          for c in range(8):
                              T.mm(P[2 + fc][:, :], wub[bi][:, c, fc * 128:(fc + 1) * 128], h2T[:, c, cs], start=(c == 0), stop=(c == 7))
                      T.mm(P[4][:, :], selst[:, e_i, :], combT[:, cs])
                      if pend is not None:
                          down(*pend)
                      hd = hid[it % 2]
                      it += 1
                      for fc in range(2):
                          T.act(sg[:, fc, :], P[fc][:, :], AF.Silu)
                          T.tt(sg[:, fc, :], sg[:, fc, :], P[2 + fc][:, :], ALU.mult)
                          T.tt(hd[:, fc, :], sg[:, fc, :], P[4][:, :], ALU.mult)
                      pend = (e_i, tc, hd, wdb[bi])
              down(*pend)
              pend = None

              ck(8, [yacc[:, 0, :], yacc[:, 9, :]])
              fg = carve(mixT_t, 0, 4096, F32)
              T.dma(fg, fg_d[:, :])
              for t in range(NT):
                  ob = xt[t % 2]
                  rs = rms_rstd(yacc[:, t, :], D, xn[:, :])
                  T.stt(ob[:, :], yacc[:, t, :], rs, fg, ALU.mult, ALU.mult)
                  T.dma(out_d[s, t * 128:(t + 1) * 128, :], ob[:, :])

        except _Done:
            pass
        T.finish()
        T.emit()
    return nc


_NC = None


def _prep_shared(inp):
    f = np.float32
    w_in = np.ascontiguousarray(inp["w_in"][0].reshape(8, 128, 3592).transpose(1, 0, 2), dtype=f)
    w_out = np.ascontiguousarray(inp["w_out"][0].reshape(8, 128, 1024).transpose(1, 0, 2), dtype=f)
    wr = np.concatenate([inp["w_group"][0], inp["w_router"][0]], axis=1)
    w_r = np.ascontiguousarray(wr.reshape(8, 128, 36).transpose(1, 0, 2), dtype=f)
    wg = np.ascontiguousarray(inp["w_gate"][0].reshape(32, 8, 128, 256).transpose(0, 2, 1, 3).reshape(32, 128, 2048), dtype=f)
    wu = np.ascontiguousarray(inp["w_up"][0].reshape(32, 8, 128, 256).transpose(0, 2, 1, 3).reshape(32, 128, 2048), dtype=f)
    wd = np.ascontiguousarray(inp["w_down"][0].reshape(32, 2, 128, 1024).transpose(0, 2, 1, 3).reshape(32, 128, 2048), dtype=f)
    gains = np.concatenate([inp["attn_norm"][0].reshape(8, 128).T, inp["ffn_norm"][0].reshape(8, 128).T], axis=1)
    fg = np.broadcast_to(inp["final_norm"][None, :], (128, D))
    gn4 = np.broadcast_to(np.tile(inp["gdn_norm"][0], 4)[None, :], (128, 512))
    cwv = inp["conv_w"][0].reshape(4, 12, 128).transpose(2, 1, 0).reshape(128, 48)
    dtb = np.broadcast_to(np.tile(inp["dt_bias"][0], 16)[None, :], (128, 64))
    alog = np.broadcast_to(np.tile(inp["A_log"][0], 16)[None, :], (128, 64))
    kind = (np.arange(S)[None, :] // 256 == np.arange(8)[:, None])
    sele = np.zeros((32, 32, 128), f)
    for e in range(32):
        sele[e, e, :] = 1.0
    c = lambda a: np.ascontiguousarray(a, dtype=f)
    return {"w_in": w_in, "w_out": w_out, "w_r": w_r, "w_gate": wg, "w_up": wu, "w_down": wd,
            "cm": _const_mats(), "ac": _attn_consts(), "kind": c(kind), "gains": c(gains), "fgain": c(fg),
            "gn4": c(gn4), "cw": c(cwv), "dtb": c(dtb), "alog": c(alog), "sele": c(sele.reshape(32, 4096))}


def kernel(**inputs):
    global _NC
    inp = {k: np.asarray(v) for k, v in inputs.items()}
    if _NC is None:
        _NC = build_program()
    shared = _prep_shared(inp)
    x = np.ascontiguousarray(inp["x"], dtype=np.float32)
    in_maps = []
    for c in range(8):
        m = dict(shared)
        m["x"] = np.ascontiguousarray(x[c * NSEQ:(c + 1) * NSEQ])
        in_maps.append(m)
    res = run_bass_kernel_spmd(_NC, in_maps, core_ids=list(range(8)))
    out = np.concatenate([np.asarray(r["out"]) for r in res.results], axis=0)
    return out.astype(np.float32, copy=False)
```
